# Optimizing a Trainium2 kernel written in Bass

```python
import jax
import jax.numpy as jnp
from jax import lax
import numpy as np

D_MODEL = 1024
BATCH = 4
SEQ = 8192
DEPTH = 2

CHUNK = 64
EPS = 1e-6
CONV_W = 4
N_BRANCH = 4
BR_WIDTH = D_MODEL // 2

RG_BLOCKS = 8
RG_BLOCK = BR_WIDTH // RG_BLOCKS
RG_C = 8.0

SSD_HEAD_DIM = 64
SSD_HEADS = BR_WIDTH // SSD_HEAD_DIM
SSD_GROUPS = 2
SSD_HPG = SSD_HEADS // SSD_GROUPS
SSD_STATE = 128
SSD_XBC = BR_WIDTH + 2 * SSD_GROUPS * SSD_STATE

ML_HEADS = 4
ML_HEAD_DIM = BR_WIDTH // ML_HEADS

RET_HEADS = 4
RET_QK_DIM = 64
RET_V_DIM = BR_WIDTH // RET_HEADS
ROPE_BASE = 10000.0

IN_WIDTHS = (
    BR_WIDTH, BR_WIDTH,
    SSD_XBC, BR_WIDTH, SSD_HEADS,
    BR_WIDTH, BR_WIDTH, BR_WIDTH, BR_WIDTH, BR_WIDTH,
    ML_HEADS, ML_HEADS,
    RET_HEADS * RET_QK_DIM, RET_HEADS * RET_QK_DIM,
    BR_WIDTH, BR_WIDTH,
    N_BRANCH * D_MODEL,
)
W_IN = sum(IN_WIDTHS)
SPLIT_POINTS = tuple(int(v) for v in np.cumsum(IN_WIDTHS)[:-1])

kernel_name = 'hybrid_rglru_ssd_mlstm_retention_encoder'


def rmsnorm(x, g):
    xf = x.astype(jnp.float32)
    y = xf * lax.rsqrt(jnp.mean(xf * xf, axis=-1, keepdims=True) + EPS)
    return (y * g.astype(jnp.float32)).astype(x.dtype)


def headwise_rmsnorm(y, g, n_heads):
    b, s, w = y.shape
    yh = y.reshape(b, s, n_heads, w // n_heads).astype(jnp.float32)
    yh = yh * lax.rsqrt(jnp.mean(yh * yh, axis=-1, keepdims=True) + EPS)
    return yh.reshape(b, s, w) * g.astype(jnp.float32)


def causal_depthwise_conv(x, w, b):
    s = x.shape[1]
    xp = jnp.pad(x, ((0, 0), (CONV_W - 1, 0), (0, 0)))
    out = b + w[CONV_W - 1] * x
    for j in range(CONV_W - 1):
        out = out + w[j] * xp[:, j:j + s]
    return out


def causal_mask(n):
    return jnp.tril(jnp.ones((n, n), dtype=bool))


def segsum_exp(a_cs):
    diff = a_cs[..., :, None] - a_cs[..., None, :]
    return jnp.exp(jnp.where(causal_mask(a_cs.shape[-1]), diff, -jnp.inf))


def chunk_heads(t, n_heads):
    b, s, w = t.shape
    return t.reshape(b, s // CHUNK, CHUNK, n_heads, w // n_heads).transpose(0, 3, 1, 2, 4).astype(jnp.float32)


def chunk_gates(t, n_heads):
    b, s, _ = t.shape
    return t.reshape(b, s // CHUNK, CHUNK, n_heads).transpose(0, 3, 1, 2).astype(jnp.float32)


def rope(x, pos):
    half = x.shape[-1] // 2
    inv = ROPE_BASE ** (-jnp.arange(half, dtype=jnp.float32) / half)
    ang = pos.astype(jnp.float32)[:, None] * inv
    cos = jnp.cos(ang)[None, :, None, :]
    sin = jnp.sin(ang)[None, :, None, :]
    x1, x2 = x[..., :half], x[..., half:]
    return jnp.concatenate([x1 * cos - x2 * sin, x1 * sin + x2 * cos], axis=-1)


def rglru_branch(u, z, conv_w, conv_b, wa, ba, wx, bx, lam):
    bsz, s, _ = u.shape
    xc = causal_depthwise_conv(u, conv_w, conv_b)
    xb = xc.reshape(bsz, s, RG_BLOCKS, RG_BLOCK)
    r = jax.nn.sigmoid(jnp.einsum('bshi,hij->bshj', xb, wa).reshape(bsz, s, BR_WIDTH) + ba)
    i = jax.nn.sigmoid(jnp.einsum('bshi,hij->bshj', xb, wx).reshape(bsz, s, BR_WIDTH) + bx)
    log_a = -RG_C * r.astype(jnp.float32) * jax.nn.softplus(-lam.astype(jnp.float32))
    a = jnp.exp(log_a)
    drive = jnp.sqrt(-jnp.expm1(2.0 * log_a)) * (i * xc).astype(jnp.float32)

    def combine(left, right):
        a1, b1 = left
        a2, b2 = right
        return a1 * a2, a2 * b1 + b2

    _, h = lax.associative_scan(combine, (a, drive), axis=1)
    return h * jax.nn.silu(z.astype(jnp.float32))


def ssd_branch(xbc_raw, z, dt_raw, conv_w, conv_b, dt_bias, a_log, d_skip, norm_g):
    bsz, s, _ = z.shape
    nc = s // CHUNK
    xbc = jax.nn.silu(causal_depthwise_conv(xbc_raw, conv_w, conv_b)).astype(jnp.float32)
    gn = SSD_GROUPS * SSD_STATE
    xh = xbc[..., :BR_WIDTH].reshape(bsz, nc, CHUNK, SSD_GROUPS, SSD_HPG, SSD_HEAD_DIM)
    bm = xbc[..., BR_WIDTH:BR_WIDTH + gn].reshape(bsz, nc, CHUNK, SSD_GROUPS, SSD_STATE)
    cm = xbc[..., BR_WIDTH + gn:].reshape(bsz, nc, CHUNK, SSD_GROUPS, SSD_STATE)
    dt = jax.nn.softplus(dt_raw.astype(jnp.float32) + dt_bias.astype(jnp.float32))
    a_dt = -jnp.exp(a_log.astype(jnp.float32)) * dt
    x_dt = xh * dt.reshape(bsz, nc, CHUNK, SSD_GROUPS, SSD_HPG)[..., None]
    a_cs = jnp.cumsum(a_dt.reshape(bsz, nc, CHUNK, SSD_GROUPS, SSD_HPG).transpose(0, 3, 4, 1, 2), axis=-1)
    cb = jnp.einsum('bclgn,bcsgn->bgcls', cm, bm)
    y_diag = jnp.einsum('bgcls,bgecls,bcsgep->bclgep', cb, segsum_exp(a_cs), x_dt)
    decay_to_end = jnp.exp(a_cs[..., -1:] - a_cs)
    states = jnp.einsum('bclgn,bgecl,bclgep->bcgepn', bm, decay_to_end, x_dt)
    chunk_decay = jnp.exp(a_cs[..., -1])

    def step(h, inp):
        dec, st = inp
        return dec[..., None, None] * h + st, h

    h0 = jnp.zeros((bsz, SSD_GROUPS, SSD_HPG, SSD_HEAD_DIM, SSD_STATE), jnp.float32)
    _, prev = lax.scan(step, h0, (jnp.moveaxis(chunk_decay, -1, 0), jnp.moveaxis(states, 1, 0)))
    y_off = jnp.einsum('bclgn,cbgepn,bgecl->bclgep', cm, prev, jnp.exp(a_cs))
    y = y_diag + y_off + d_skip.astype(jnp.float32).reshape(SSD_GROUPS, SSD_HPG)[:, :, None] * xh
    y = y.reshape(bsz, s, BR_WIDTH)
    return rmsnorm(y * jax.nn.silu(z.astype(jnp.float32)), norm_g)


def mlstm_branch(q, k, v, o_raw, z, i_raw, f_raw, i_bias, f_bias, norm_g):
    bsz, s, _ = q.shape
    qh = chunk_heads(q, ML_HEADS)
    kh = chunk_heads(k, ML_HEADS) * (ML_HEAD_DIM ** -0.5)
    vh = chunk_heads(v, ML_HEADS)
    log_i = chunk_gates(i_raw, ML_HEADS) + i_bias.astype(jnp.float32)[None, :, None, None]
    log_f = jax.nn.log_sigmoid(chunk_gates(f_raw, ML_HEADS) + f_bias.astype(jnp.float32)[None, :, None, None])
    f_cs = jnp.cumsum(log_f, axis=-1)
    f_tot = f_cs[..., -1]
    w_end = f_tot[..., None] - f_cs + log_i
    m_loc = jnp.max(w_end, axis=-1)
    p_end = jnp.exp(w_end - m_loc[..., None])
    c_loc = jnp.einsum('bhcl,bhcld,bhcle->bhcde', p_end, vh, kh)
    n_loc = jnp.einsum('bhcl,bhcle->bhce', p_end, kh)

    def step(carry, inp):
        c_st, n_st, m_st = carry
        ft, ml, cl, nl = inp
        m_new = jnp.maximum(ft + m_st, ml)
        s_old = jnp.exp(ft + m_st - m_new)
        s_loc = jnp.exp(ml - m_new)
        c_new = s_old[..., None, None] * c_st + s_loc[..., None, None] * cl
        n_new = s_old[..., None] * n_st + s_loc[..., None] * nl
        return (c_new, n_new, m_new), (c_st, n_st, m_st)

    init = (jnp.zeros((bsz, ML_HEADS, ML_HEAD_DIM, ML_HEAD_DIM), jnp.float32),
            jnp.zeros((bsz, ML_HEADS, ML_HEAD_DIM), jnp.float32),
            jnp.zeros((bsz, ML_HEADS), jnp.float32))
    _, (c_prev, n_prev, m_prev) = lax.scan(
        step, init, (jnp.moveaxis(f_tot, 2, 0), jnp.moveaxis(m_loc, 2, 0),
                     jnp.moveaxis(c_loc, 2, 0), jnp.moveaxis(n_loc, 2, 0)))
    m_prev = jnp.moveaxis(m_prev, 0, 2)
    log_d = f_cs[..., :, None] - f_cs[..., None, :] + log_i[..., None, :]
    log_d = jnp.where(causal_mask(CHUNK), log_d, -jnp.inf)
    log_a = f_cs + m_prev[..., None]
    m_row = jnp.maximum(log_a, jnp.max(log_d, axis=-1))
    scores = jnp.einsum('bhcld,bhcsd->bhcls', qh, kh) * jnp.exp(log_d - m_row[..., None])
    scale_prev = jnp.exp(log_a - m_row)
    num = jnp.einsum('bhcls,bhcsd->bhcld', scores, vh) + scale_prev[..., None] * jnp.einsum('cbhde,bhcle->bhcld', c_prev, qh)
    den = jnp.sum(scores, axis=-1) + scale_prev * jnp.einsum('cbhe,bhcle->bhcl', n_prev, qh)
    h = num / jnp.maximum(jnp.abs(den), jnp.exp(-m_row))[..., None]
    h = h.transpose(0, 2, 3, 1, 4).reshape(bsz, s, BR_WIDTH)
    h = jax.nn.sigmoid(o_raw.astype(jnp.float32)) * h
    return headwise_rmsnorm(h, norm_g, ML_HEADS) * jax.nn.silu(z.astype(jnp.float32))


def retention_branch(q, k, v, z, norm_g):
    bsz, s, _ = q.shape
    pos = jnp.arange(s)
    qr = rope(q.reshape(bsz, s, RET_HEADS, RET_QK_DIM).astype(jnp.float32), pos)
    kr = rope(k.reshape(bsz, s, RET_HEADS, RET_QK_DIM).astype(jnp.float32), pos) * (RET_QK_DIM ** -0.5)
    qh = chunk_heads(qr.reshape(bsz, s, -1), RET_HEADS)
    kh = chunk_heads(kr.reshape(bsz, s, -1), RET_HEADS)
    vh = chunk_heads(v, RET_HEADS)
    log_g = jnp.log1p(-jnp.exp2(-5.0 - jnp.arange(RET_HEADS, dtype=jnp.float32)))
    idx = jnp.arange(CHUNK, dtype=jnp.float32)
    rel = idx[:, None] - idx[None, :]
    dmat = jnp.where(rel >= 0, jnp.exp(log_g[:, None, None] * jnp.maximum(rel, 0.0)), 0.0)
    inner = jnp.einsum('bhcld,bhcsd->bhcls', qh, kh) * dmat[None, :, None]
    y_in = jnp.einsum('bhcls,bhcse->bhcle', inner, vh)
    dec_end = jnp.exp(log_g[:, None] * (CHUNK - 1.0 - idx))
    r_loc = jnp.einsum('bhcld,hl,bhcle->bhcde', kh, dec_end, vh)
    chunk_dec = jnp.exp(log_g * CHUNK)

    def step(r_st, r_c):
        return chunk_dec[None, :, None, None] * r_st + r_c, r_st

    r0 = jnp.zeros((bsz, RET_HEADS, RET_QK_DIM, RET_V_DIM), jnp.float32)
    _, r_prev = lax.scan(step, r0, jnp.moveaxis(r_loc, 2, 0))
    dec_start = jnp.exp(log_g[:, None] * (idx + 1.0))
    y = y_in + jnp.einsum('bhcld,cbhde,hl->bhcle', qh, r_prev, dec_start)
    y = y.transpose(0, 2, 3, 1, 4).reshape(bsz, s, BR_WIDTH)
    return headwise_rmsnorm(y, norm_g, RET_HEADS) * jax.nn.silu(z.astype(jnp.float32))


def hybrid_layer(x, norm_g, w_in, a_conv_w, a_conv_b, a_gate_a_w, a_gate_a_b, a_gate_x_w,
                 a_gate_x_b, a_lambda, b_conv_w, b_conv_b, b_dt_bias, b_a_log, b_d_skip,
                 b_norm_g, c_i_bias, c_f_bias, c_norm_g, d_norm_g, w_branch, w_out):
    bsz, s, _ = x.shape
    hn = rmsnorm(x, norm_g)
    proj = jnp.einsum('bsd,dw->bsw', hn, w_in)
    (a_x, a_z, b_xbc, b_z, b_dt, c_q, c_k, c_v, c_o, c_z, c_i, c_f,
     d_q, d_k, d_v, d_z, gates) = jnp.split(proj, SPLIT_POINTS, axis=-1)
    y_a = rglru_branch(a_x, a_z, a_conv_w, a_conv_b, a_gate_a_w, a_gate_a_b, a_gate_x_w, a_gate_x_b, a_lambda)
    y_b = ssd_branch(b_xbc, b_z, b_dt, b_conv_w, b_conv_b, b_dt_bias, b_a_log, b_d_skip, b_norm_g)
    y_c = mlstm_branch(c_q, c_k, c_v, c_o, c_z, c_i, c_f, c_i_bias, c_f_bias, c_norm_g)
    y_d = retention_branch(d_q, d_k, d_v, d_z, d_norm_g)
    branches = jnp.stack([y_a, y_b, y_c, y_d], axis=2).astype(x.dtype)
    up = jnp.einsum('bsnw,nwd->bsnd', branches, w_branch)
    g = jax.nn.sigmoid(gates.reshape(bsz, s, N_BRANCH, D_MODEL))
    merged = jnp.sum(g * up, axis=2)
    return x + jnp.einsum('bsd,de->bse', merged, w_out)


def setup_inputs(seed: int = 0) -> dict:
    key = jax.random.key(seed)
    ks = jax.random.split(key, 24)
    f32 = jnp.float32

    def nrm(k, shape, scale):
        return jax.random.normal(k, shape, f32) * scale

    L = DEPTH
    u_lam = jax.random.uniform(ks[9], (L, BR_WIDTH), f32, 0.9, 0.999)
    base = u_lam ** (1.0 / RG_C)
    dt0 = jnp.exp(jax.random.uniform(ks[12], (L, SSD_HEADS), f32, np.log(0.001), np.log(0.1)))
    return {
        'x': nrm(ks[0], (BATCH, SEQ, D_MODEL), 1.0),
        'norm_g': 1.0 + nrm(ks[1], (L, D_MODEL), 0.02),
        'w_in': nrm(ks[2], (L, D_MODEL, W_IN), D_MODEL ** -0.5),
        'a_conv_w': nrm(ks[3], (L, CONV_W, BR_WIDTH), CONV_W ** -0.5),
        'a_conv_b': nrm(ks[4], (L, BR_WIDTH), 0.02),
        'a_gate_a_w': nrm(ks[5], (L, RG_BLOCKS, RG_BLOCK, RG_BLOCK), RG_BLOCK ** -0.5),
        'a_gate_a_b': nrm(ks[6], (L, BR_WIDTH), 0.02),
        'a_gate_x_w': nrm(ks[7], (L, RG_BLOCKS, RG_BLOCK, RG_BLOCK), RG_BLOCK ** -0.5),
        'a_gate_x_b': nrm(ks[8], (L, BR_WIDTH), 0.02),
        'a_lambda': jnp.log(base) - jnp.log1p(-base),
        'b_conv_w': nrm(ks[10], (L, CONV_W, SSD_XBC), CONV_W ** -0.5),
        'b_conv_b': nrm(ks[11], (L, SSD_XBC), 0.02),
        'b_dt_bias': dt0 + jnp.log(-jnp.expm1(-dt0)),
        'b_a_log': jnp.log(jax.random.uniform(ks[13], (L, SSD_HEADS), f32, 1.0, 16.0)),
        'b_d_skip': 1.0 + nrm(ks[14], (L, SSD_HEADS), 0.1),
        'b_norm_g': 1.0 + nrm(ks[15], (L, BR_WIDTH), 0.02),
        'c_i_bias': nrm(ks[16], (L, ML_HEADS), 0.1),
        'c_f_bias': 3.0 + nrm(ks[17], (L, ML_HEADS), 0.5),
        'c_norm_g': 1.0 + nrm(ks[18], (L, BR_WIDTH), 0.02),
        'd_norm_g': 1.0 + nrm(ks[19], (L, BR_WIDTH), 0.02),
        'w_branch': nrm(ks[20], (L, N_BRANCH, BR_WIDTH, D_MODEL), BR_WIDTH ** -0.5),
        'w_out': nrm(ks[21], (L, D_MODEL, D_MODEL), D_MODEL ** -0.5),
        'final_norm_g': 1.0 + nrm(ks[22], (D_MODEL,), 0.02),
    }


def reference(x, norm_g, w_in, a_conv_w, a_conv_b, a_gate_a_w, a_gate_a_b, a_gate_x_w,
              a_gate_x_b, a_lambda, b_conv_w, b_conv_b, b_dt_bias, b_a_log, b_d_skip,
              b_norm_g, c_i_bias, c_f_bias, c_norm_g, d_norm_g, w_branch, w_out, final_norm_g):
    for l in range(DEPTH):
        x = hybrid_layer(x, norm_g[l], w_in[l], a_conv_w[l], a_conv_b[l], a_gate_a_w[l],
                         a_gate_a_b[l], a_gate_x_w[l], a_gate_x_b[l], a_lambda[l],
                         b_conv_w[l], b_conv_b[l], b_dt_bias[l], b_a_log[l], b_d_skip[l],
                         b_norm_g[l], c_i_bias[l], c_f_bias[l], c_norm_g[l], d_norm_g[l],
                         w_branch[l], w_out[l])
    return rmsnorm(x, final_norm_g)
```

```python
import math
import numpy as np
import concourse.bass as bass
import concourse.mybir as mybir
from concourse.bass_utils import run_bass_kernel_spmd

F32 = mybir.dt.float32
BF16 = mybir.dt.bfloat16
I32 = mybir.dt.int32
AF = mybir.ActivationFunctionType
ALU = mybir.AluOpType

L = 2
D = 1024
EPS = 1e-6
NBLK = 27
NSLOT = 6
W_IN = 10768
C_AX, C_AZ, C_BX, C_BZ, C_BDT = 0, 512, 1024, 2048, 2560
C_CQ, C_CK, C_CV, C_CO, C_CZ, C_CI, C_CF = 2568, 3080, 3592, 4104, 4616, 5128, 5132
C_DQ, C_DK, C_DV, C_DZ, C_G = 5136, 5392, 5648, 6160, 6672
BLK_COLS = [C_AX, C_AZ, C_BX, C_BX + 512, C_BZ, C_CQ, C_CK, C_CZ, C_CV, C_CO, C_DZ, C_DQ, C_DV]
(B_AX, B_AZ, B_XBC0, B_XBC1, B_BZ, B_CQ, B_CK, B_CZ, B_CV, B_CO, B_DZ, B_DQK, B_DV) = range(13)
B_WOUT = 25


def blk_gate(n, jb):
    return 13 + jb * 6 + [0, 2, 3, 5][n]


def blk_wb(pair, jb):
    return 13 + jb * 6 + [1, 4][pair]


def prm_layout():
    off = {}
    p = 0

    def add(name, n):
        nonlocal p
        off[name] = (p, n)
        p += n

    for l in range(L):
        add(f"g{l}", 8)
        add(f"a_cw{l}", 16)
        add(f"a_cb{l}", 4)
        add(f"a_ba{l}", 4)
        add(f"a_bx{l}", 4)
        add(f"a_lam{l}", 4)
        add(f"a_wa{l}", 512)
        add(f"a_wx{l}", 512)
        add(f"b_cw{l}", 32)
        add(f"b_cb{l}", 8)
        add(f"smb{l}", 16)
        add(f"b_alog{l}", 8)
        add(f"b_dsk{l}", 8)
        add(f"b_g{l}", 4)
        add(f"c_g{l}", 4)
        add(f"d_g{l}", 4)
        add(f"wsm{l}", 128)
    add("gf", 1024)
    return off, p


PRM_OFF, NPRM = prm_layout()


def pack_params(inp):
    prm = np.zeros((128, NPRM), np.float32)

    def put(name, arr):
        o, n = PRM_OFF[name]
        prm[:, o:o + n] = np.asarray(arr, np.float32).reshape(128, n)

    def fm(v, nch):
        return np.asarray(v).reshape(nch, 128).T

    def bc(v):
        return np.broadcast_to(np.asarray(v)[None, :], (128, len(v)))

    for l in range(L):
        put(f"g{l}", fm(inp["norm_g"][l], 8))
        put(f"a_cw{l}", inp["a_conv_w"][l].reshape(4, 4, 128).transpose(2, 1, 0))
        put(f"a_cb{l}", fm(inp["a_conv_b"][l], 4))
        put(f"a_ba{l}", fm(inp["a_gate_a_b"][l], 4))
        put(f"a_bx{l}", fm(inp["a_gate_x_b"][l], 4))
        put(f"a_lam{l}", fm(inp["a_lambda"][l], 4))
        for nm, src in ((f"a_wa{l}", inp["a_gate_a_w"][l]), (f"a_wx{l}", inp["a_gate_x_w"][l])):
            m = np.zeros((128, 4, 128), np.float32)
            for c in range(4):
                m[0:64, c, 0:64] = src[2 * c]
                m[64:128, c, 64:128] = src[2 * c + 1]
            put(nm, m)
        put(f"b_cw{l}", inp["b_conv_w"][l].reshape(4, 8, 128).transpose(2, 1, 0))
        put(f"b_cb{l}", fm(inp["b_conv_b"][l], 8))
        put(f"smb{l}", bc(np.concatenate([inp["b_dt_bias"][l], inp["c_i_bias"][l], inp["c_f_bias"][l]])))
        put(f"b_alog{l}", bc(inp["b_a_log"][l]))
        put(f"b_dsk{l}", bc(inp["b_d_skip"][l]))
        put(f"b_g{l}", fm(inp["b_norm_g"][l], 4))
        put(f"c_g{l}", fm(inp["c_norm_g"][l], 4))
        put(f"d_g{l}", fm(inp["d_norm_g"][l], 4))
        cols = list(range(C_BDT, C_BDT + 8)) + list(range(C_CI, C_CI + 8))
        put(f"wsm{l}", inp["w_in"][l][:, cols].reshape(8, 128, 16).transpose(1, 0, 2))
    put("gf", bc(inp["final_norm_g"]))
    return prm


def pack_weights(inp):
    wst = np.empty((L, NBLK, 128, 4096), np.float32)
    for l in range(L):
        w_in = inp["w_in"][l]

        def inblk(c0):
            return w_in[:, c0:c0 + 512].reshape(8, 128, 512).transpose(1, 0, 2).reshape(128, 4096)

        for b, c0 in enumerate(BLK_COLS):
            wst[l, b] = inblk(c0)
        for jb in range(2):
            for n in range(4):
                wst[l, blk_gate(n, jb)] = inblk(C_G + n * 1024 + jb * 512)
            for pair in range(2):
                m = np.stack([inp["w_branch"][l, 2 * pair + i][:, jb * 512:(jb + 1) * 512]
                              .reshape(4, 128, 512).transpose(1, 0, 2) for i in range(2)], axis=1)
                wst[l, blk_wb(pair, jb)] = m.reshape(128, 4096)
        for hf in range(2):
            wst[l, B_WOUT + hf] = inp["w_out"][l][:, hf * 512:(hf + 1) * 512] \
                .reshape(8, 128, 512).transpose(1, 0, 2).reshape(128, 4096)
    return wst.reshape(L * NBLK * 256, 2048)


import re
_SCR = re.compile(r"(hr\d|fr\d|big|tiny)")


class Em:
    ENG = ("pe", "act", "dve", "pool", "sp")

    def __init__(self, nc):
        self.nc = nc
        self.e = {"pe": nc.tensor, "act": nc.scalar, "dve": nc.vector, "pool": nc.gpsimd, "sp": nc.sync}
        self.sem = {k: nc.alloc_semaphore("sem_" + k) for k in self.ENG}
        self.cnt = {k: 0 for k in self.ENG}
        self.dsem = {}
        self.dcnt = {}
        self.seen = {k: {} for k in self.ENG}
        self.last_w = {}
        self.readers = {}
        self.gen = {}
        self.sfx = ""
        self.ninst = 0
        self.nwait = 0

    def _canon(self, k):
        if self.sfx and _SCR.fullmatch(k):
            return k + self.sfx
        if "#" in k:
            base, g = k.split("#")
            assert self.gen.get(base) == int(g), f"stale psum handle {k} (cur {self.gen.get(base)})"
            return base
        return k

    def _sem_of(self, s):
        return self.sem[s] if s in self.sem else self.dsem[s]

    def _need(self, eng, deps):
        best = {}
        for d in deps:
            if d is None:
                continue
            s, v, pe, raw = d
            if pe == eng and (eng == "pe" or not raw):
                continue
            if self.seen[eng].get(s, 0) >= v:
                continue
            if best.get(s, 0) < v:
                best[s] = v
        for s, v in best.items():
            self.e[eng].wait_ge(self._sem_of(s), v)
            self.seen[eng][s] = v
            self.nwait += 1

    def _deps(self, R, W):
        deps = []
        for k in R:
            d = self.last_w.get(k)
            if d is not None:
                deps.append(d + (True,))
        for k in W:
            d = self.last_w.get(k)
            if d is not None:
                deps.append(d + (False,))
            deps.extend(r + (False,) for r in self.readers.get(k, ()))
        return deps

    def _mark(self, tag, R, W):
        for k in W:
            self.last_w[k] = tag
            self.readers[k] = []
        for k in R:
            if k not in W:
                self.readers.setdefault(k, []).append(tag)

    def op(self, eng, fn, R=(), W=()):
        R = [self._canon(k) for k in R]
        W = [self._canon(k) for k in W]
        W = W + [k for k in R if k.startswith("ps") and k not in W]
        R = [k for k in R if not k.startswith("ps")]
        self._need(eng, self._deps(R, W))
        ins = fn(self.e[eng])
        self.cnt[eng] += 1
        ins.then_inc(self.sem[eng], 1)
        self._mark((eng, self.cnt[eng], eng), R, W)
        self.ninst += 1

    def dma(self, q, out, in_, stream, R=(), W=(), **kw):
        R = [self._canon(k) for k in R]
        W = [self._canon(k) for k in W]
        self._need(q, self._deps(R, W))
        if stream not in self.dsem:
            self.dsem[stream] = self.nc.alloc_semaphore("dsem_" + stream)
            self.dcnt[stream] = 0
        ins = self.e[q].dma_start(out=out, in_=in_, **kw)
        self.dcnt[stream] += 16
        ins.then_inc(self.dsem[stream], 16)
        self._mark((stream, self.dcnt[stream], None), R, W)
        self.ninst += 1

    def finish(self, eng="sp"):
        deps = [(s, v, None, True) for s, v in self.dcnt.items() if v > 0]
        deps += [(k, v, k, True) for k, v in self.cnt.items() if v > 0 and k != eng]
        self._need(eng, deps)


class _Stop(Exception):
    pass


def build(NT, dbg=False, stop=None):
    try:
        return _build(NT, dbg, stop)
    except _Stop as e:
        nc, em = e.args
        em.finish("sp")
        print("STOPPED at", stop, "instructions", em.ninst)
        return nc


def _build(NT, dbg=False, stop=None):
    nc = bass.Bass("TRN2", target_bir_lowering=False)
    S = NT * 512
    x_d = nc.dram_tensor("x", [S, D], F32, kind="ExternalInput")
    wst_d = nc.dram_tensor("wst", [L * NBLK * 256, 2048], F32, kind="ExternalInput")
    prm_d = nc.dram_tensor("prm", [128, NPRM], F32, kind="ExternalInput")
    y_d = nc.dram_tensor("y", [S, D], F32, kind="ExternalOutput")
    wsb_d = nc.dram_tensor("wsb", [L * NBLK, 128, 4096], BF16)
    rope_d = nc.dram_tensor("rope", [NT, 128, 256], F32)
    if dbg:
        dbg_d = nc.dram_tensor("dbg", [NT * L, 128, 8192], BF16, kind="ExternalOutput")
    em = Em(nc)
    if dbg:
        dbg2_d = nc.dram_tensor("dbg2", [16, 128, 512], F32, kind="ExternalOutput")
    ddn = [0]

    def dd(ap, keys, bf=False):
        if not dbg or ddn[0] >= 16:
            return
        i = ddn[0]
        ddn[0] += 1
        if bf:
            em.op("dve", lambda e: e.tensor_copy(ddt[:], ap), keys, ["ddt"])
            em.dma("sp", dbg2_d.ap()[i], ddt[:], "dbg2", R=["ddt"])
        else:
            em.dma("sp", dbg2_d.ap()[i], ap, "dbg2", R=keys)
        print("dd slot", i, keys)

    def SB(name, shape, dt=F32):
        return nc.alloc_sbuf_tensor("s_" + name, shape, dt)

    ddt = SB("ddt", [128, 512]) if dbg else None
    prm = SB("prm", [128, NPRM])
    xres = SB("xres", [128, 4, D])
    xs = SB("xs", [128, D], BF16)
    hnT = SB("hnT", [128, 8, 512], BF16)
    wsl = [SB(f"wsl{i}", [128, 4096], BF16) for i in range(NSLOT)]
    yT = SB("yT", [128, 4, 4, 512], BF16)
    arena = SB("arena", [128, 6144], BF16)
    FR = [SB(f"fr{i}", [128, 512]) for i in range(8)]
    HR = [SB(f"hr{i}", [128, 512], BF16) for i in range(10)]
    big = SB("big", [128, 1024])
    HR2 = [SB(f"hrb{i}", [128, 512], BF16) for i in range(8)]
    FR2 = [SB(f"frb{i}", [128, 512]) for i in range(7)]
    big2 = SB("big2", [128, 1024])
    tiny2 = SB("tiny2", [128, 128])
    arenaB = SB("arenaB", [128, 4096], BF16)
    FRP = SB("frp", [128, 512])
    ident = SB("ident", [128, 128], BF16)
    Vm = SB("Vm", [128, 128])
    Um = SB("Um", [128, 128])
    ones = SB("ones", [128, 128])
    Vb = SB("Vb", [128, 128], BF16)
    onesb = SB("onesb", [128, 2], BF16)
    cst = SB("cst", [128, 64])
    sm = SB("sm", [128, 4, 16])
    sp_ = SB("sp", [128, 4, 12])
    adt = SB("adt", [128, 4, 8])
    tiny = SB("tiny", [128, 128])
    ropeT = SB("ropeT", [128, 4, 64])
    wsmb = SB("wsmb", [128, L, 8, 16], BF16)
    lay = SB("lay", [128, L, 32])
    a_halo = SB("a_halo", [128, L, 4, 3])
    a_h = SB("a_h", [128, L, 4])
    b_halo = SB("b_halo", [128, L, 8, 3])
    b_st = SB("b_st", [128, L, 512])
    b_stb = SB("b_stb", [128, L, 512], BF16)
    c_st = SB("c_st", [128, L, 4, 130])
    c_stb = SB("c_stb", [128, L, 4, 130], BF16)
    d_st = SB("d_st", [128, L, 2, 128])
    d_stb = SB("d_stb", [128, L, 2, 128], BF16)
    print("sbuf remaining", nc.sbuf_bytes_remaining)

    PS = [nc.alloc_psum_tensor(f"ps{i}", [128, 512], F32) for i in range(8)]
    def mk_bank(lo, hi):
        st = [0]

        def f():
            i = lo + st[0] % (hi - lo)
            st[0] += 1
            g = em.gen.get(f"ps{i}", 0) + 1
            em.gen[f"ps{i}"] = g
            return PS[i], f"ps{i}#{g}"
        return f

    bank = mk_bank(0, 8)
    BANKF = [mk_bank(0, 4), mk_bank(4, 8)]

    def P(name, n=None):
        o, m = PRM_OFF[name]
        return prm[:, o:o + (m if n is None else n)]

    def TT(eng, out, a, b, op, R, W):
        em.op(eng, lambda e: e.tensor_tensor(out=out, in0=a, in1=b, op=op), R, W)

    def TS(eng, out, a, s1, s2, op0, op1, R, W):
        if s2 is None:
            em.op(eng, lambda e: e.tensor_scalar(out, a, s1, None, op0), R, W)
        else:
            em.op(eng, lambda e: e.tensor_scalar(out, a, s1, s2, op0, op1), R, W)

    def STT(eng, out, a, s, b, op0, op1, R, W):
        em.op(eng, lambda e: e.scalar_tensor_tensor(out=out, in0=a, scalar=s, in1=b, op0=op0, op1=op1), R, W)

    def CP(eng, out, a, R, W):
        if eng == "act":
            em.op(eng, lambda e: e.copy(out, a), R, W)
        else:
            em.op(eng, lambda e: e.tensor_copy(out, a), R, W)

    def ACT(out, a, func, R, W, bias=None, scale=None, accum=None):
        kw = {}
        if bias is not None:
            kw["bias"] = bias
        if scale is not None:
            kw["scale"] = scale
        if accum is not None:
            kw["accum_out"] = accum
        em.op("act", lambda e: e.activation(out=out, in_=a, func=func, **kw), R, W)

    def MM(out, lhsT, rhs, start, stop, R, W):
        em.op("pe", lambda e: e.matmul(out, lhsT=lhsT, rhs=rhs, start=start, stop=stop), R, W)

    def TR(out, a, R, W):
        em.op("pe", lambda e: e.transpose(out, a, ident[:]), R + ["ident"], W)

    def MS(eng, ap, val, W):
        em.op(eng, lambda e: e.memset(ap, val), [], W)

    class WS:
        def __init__(self):
            self.nissued = 0
            self.done = set()
            self.total = NT * L * NBLK

        def pump(self):
            while self.nissued < self.total:
                j = self.nissued
                if j >= NSLOT and (j - NSLOT) not in self.done:
                    break
                s = j % NSLOT
                lb = j % (L * NBLK)
                em.dma("sp", wsl[s][:], wsb_d.ap()[lb], f"w{s}", R=[f"wsb{lb}"], W=[f"wsl{s}"])
                self.nissued += 1

        def use(self, t, l, b):
            j = (t * L + l) * NBLK + b
            assert j < self.nissued, (j, self.nissued)
            assert j not in self.done
            s = j % NSLOT
            return wsl[s], f"wsl{s}"

        def rel(self, t, l, b):
            self.done.add((t * L + l) * NBLK + b)
            self.pump()

    ws = WS()

    em.dma("sp", prm[:], prm_d.ap(), "prm", W=["prm"])
    wsb_rows = wsb_d.ap().rearrange("b p (r f) -> b (p r) f", f=2048)
    import os
    for lb in range(0 if os.environ.get('NOCAST') else L * NBLK):
        em.dma("pool", wsb_rows[lb], wst_d.ap()[lb * 256:(lb + 1) * 256, :], f"cast{lb}", W=[f"wsb{lb}"])
    MS("pool", ones[:], 1.0, ["ones"])
    em.op("pool", lambda e: e.affine_select(out=Vm[:], in_=ones[:], pattern=[[1, 128]], compare_op=ALU.is_ge,
                                            fill=0.0, base=0, channel_multiplier=-1), ["ones"], ["Vm"])
    em.op("pool", lambda e: e.affine_select(out=Um[:], in_=ones[:], pattern=[[-1, 128]], compare_op=ALU.is_gt,
                                            fill=0.0, base=0, channel_multiplier=1), ["ones"], ["Um"])
    em.op("pool", lambda e: e.affine_select(out=ident[:], in_=ones[:], pattern=[[1, 128]], compare_op=ALU.is_equal,
                                            fill=0.0, base=0, channel_multiplier=-1), ["ones"], ["ident"])
    CP("pool", Vb[:], Vm[:], ["Vm"], ["Vb"])
    MS("pool", onesb[:], 1.0, ["onesb"])
    for (t_, k_) in ((a_halo, "a_halo"), (a_h, "a_h"), (b_halo, "b_halo"), (b_st, "b_st"), (b_stb, "b_stb"),
                     (c_st, "c_st"), (c_stb, "c_stb"), (d_st, "d_st"), (d_stb, "d_stb")):
        MS("pool", t_[:], 0.0, [k_])
    log_g = [math.log1p(-2.0 ** (-5.0 - h)) for h in range(4)]
    ci = SB("ci", [128, 64], I32)
    em.op("pool", lambda e: e.iota(ci[:, 0:1], pattern=[[0, 1]], base=1, channel_multiplier=1), [], ["ci"])
    em.op("pool", lambda e: e.iota(ci[:, 16:48], pattern=[[1, 32]], base=0, channel_multiplier=0), [], ["ci"])
    CP("dve", cst[:, 0:1], ci[:, 0:1], ["ci"], ["cst"])
    CP("dve", cst[:, 16:48], ci[:, 16:48], ["ci"], ["cst"])
    for h in range(4):
        ACT(cst[:, 1 + h:2 + h], cst[:, 0:1], AF.Exp, ["cst"], ["cst"], scale=log_g[h])
        ACT(cst[:, 5 + h:6 + h], cst[:, 0:1], AF.Exp, ["cst"], ["cst"], scale=-log_g[h])
    TS("dve", cst[:, 5:9], cst[:, 5:9], 0.125, None, ALU.mult, None, ["cst"], ["cst"])
    for j in range(2):
        MS("pool", cst[0:64, 9 + j:10 + j], math.exp(128.0 * log_g[2 * j]), ["cst"])
        MS("pool", cst[64:128, 9 + j:10 + j], math.exp(128.0 * log_g[2 * j + 1]), ["cst"])
    ACT(cst[:, 16:48], cst[:, 16:48], AF.Exp, ["cst"], ["cst"], scale=-math.log(10000.0) / 32.0)
    for l in range(L):
        ACT(lay[:, l, 0:4], P(f"a_lam{l}"), AF.Exp, ["prm"], ["lay"], scale=-1.0)
        ACT(lay[:, l, 0:4], lay[:, l, 0:4], AF.Ln, ["lay"], ["lay"], bias=1.0)
        TS("dve", lay[:, l, 4:8], lay[:, l, 0:4], -16.0, None, ALU.mult, None, ["lay"], ["lay"])
        TS("dve", lay[:, l, 0:4], lay[:, l, 0:4], -8.0, None, ALU.mult, None, ["lay"], ["lay"])
        ACT(lay[:, l, 8:16], P(f"b_alog{l}"), AF.Exp, ["prm"], ["lay"])
        TS("dve", lay[:, l, 8:16], lay[:, l, 8:16], -1.0, None, ALU.mult, None, ["lay"], ["lay"])
        CP("dve", wsmb[:, l], P(f"wsm{l}").rearrange("p (k c) -> p k c", c=16), ["prm"], ["wsmb"])
    TWO_PI = 2.0 * math.pi
    C1 = 6.28125
    C2 = TWO_PI - C1
    posi = SB("posi", [128, 4], I32)
    def rope_gen(t, RG, RK):
        em.op("pool", lambda e: e.iota(posi[:], pattern=[[128, 4]], base=t * 512, channel_multiplier=1), ["posi"], ["posi"])
        posf = tiny[:, 0:4]
        CP("dve", posf, posi[:], ["posi"], ["tiny"])
        ang = RG[0][:, 0:128].rearrange("p (b i) -> p b i", i=32)
        TT("dve", ang, posf.unsqueeze(2).to_broadcast([128, 4, 32]),
           cst[:, 16:48].unsqueeze(1).to_broadcast([128, 4, 32]), ALU.mult, ["tiny", "cst"], [RK[0]])
        for which, off in ((1, 0.0), (0, math.pi / 2)):
            a2 = RG[1][:, 0:128].rearrange("p (b i) -> p b i", i=32)
            kf = RG[2][:, 0:128].rearrange("p (b i) -> p b i", i=32)
            ki = RG[3][:, 0:128].bitcast(I32).rearrange("p (b i) -> p b i", i=32)
            TS("dve", a2, ang, off, None, ALU.add, None, [RK[0]], [RK[1]])
            TS("dve", kf, a2, 1.0 / TWO_PI, None, ALU.mult, None, [RK[1]], [RK[2]])
            CP("dve", ki, kf, [RK[2]], [RK[3]])
            CP("dve", kf, ki, [RK[3]], [RK[2]])
            STT("dve", a2, kf, -C1, a2, ALU.mult, ALU.add, [RK[2], RK[1]], [RK[1]])
            STT("dve", a2, kf, -C2, a2, ALU.mult, ALU.add, [RK[2], RK[1]], [RK[1]])
            TS("dve", a2, a2, 3.14159, -3.14159, ALU.min, ALU.max, [RK[1]], [RK[1]])
            ACT(ropeT[:, :, which * 32:(which + 1) * 32], a2, AF.Sin, [RK[1]], ["ropeT"])
        em.dma("sp", rope_d.ap()[t].rearrange("p (b i) -> p b i", i=64), ropeT[:], "ropest", R=["ropeT"], W=[f"rope{t}"])

    rope_gen(0, FR[0:4], ["fr0", "fr1", "fr2", "fr3"])

    cur = [0, 0]

    def chk(name):
        if stop == name:
            if dbg:
                em.dma("sp", dbg_d.ap()[cur[0] * L + cur[1]], yT[:].rearrange("p a b c -> p (a b c)"), "dbg",
                       R=["yT0", "yT1", "yT2", "yT3"])
            raise _Stop(nc, em)

    chk("prologue")
    ws.pump()

    def proj_fm(wt, wk, chunk, pt, pk, ncols=512, c0=0):
        for kc in range(8):
            MM(pt[:, 0:ncols], wt[:, kc * 512 + chunk * 128: kc * 512 + chunk * 128 + 128], hnT[:, kc, c0:c0 + ncols],
               kc == 0, kc == 7, [wk, "hnT"], [pk])

    def proj_tm(wt, wk, blk, pt, pk, ncols=512):
        for kc in range(8):
            MM(pt[:, 0:ncols], hnT[:, kc, blk * 128:(blk + 1) * 128], wt[:, kc * 512: kc * 512 + ncols],
               kc == 0, kc == 7, [wk, "hnT"], [pk])

    def rstd_from_ss(out, ss, n, width, R, W):
        TS("dve", out, ss, 1.0 / n, EPS, ALU.mult, ALU.add, R, W)
        ACT(out, out, AF.Sqrt, W, W)
        em.op("dve", lambda e: e.reciprocal(out=out, in_=out), W, W)

    HRS, FRS, BIGS, TINYS = [HR, HR2], [FR, FR2], [big, big2], [tiny, tiny2]

    BANKF3 = [mk_bank(0, 3), mk_bank(3, 6)]
    PBANK = mk_bank(6, 8)
    CURB = list(BANKF)

    def run_pairs(genf, extra=None):
        CURB[:] = BANKF3 if extra is not None else BANKF
        _run_pairs(genf, extra)
        if extra is not None:
            em.sfx = ""
            for _ in extra:
                pass

    def _run_pairs(genf, extra):
        for a in (0, 2):
            gens = [genf(a, 0), genf(a + 1, 1)]
            sfx = ["", "_b"]
            alive = [True, True]
            blocked1 = False
            upd0 = False
            while any(alive):
                if extra is not None:
                    em.sfx = ""
                    next(extra, None)
                for i in (0, 1):
                    if not alive[i]:
                        continue
                    if i == 1 and blocked1 and not upd0 and alive[0]:
                        continue
                    em.sfx = sfx[i]
                    try:
                        r = next(gens[i])
                    except StopIteration:
                        alive[i] = False
                        continue
                    finally:
                        em.sfx = ""
                    if i == 0 and r == "U":
                        upd0 = True
                    if i == 1 and r == "R":
                        blocked1 = True

    for t in range(NT):
        em.dma("sp", xres[:], x_d.ap()[t * 512:(t + 1) * 512, :].rearrange("(b p) d -> p b d", p=128), "xld", W=["xres"])
        em.dma("sp", ropeT[:], rope_d.ap()[t].rearrange("p (b i) -> p b i", i=64), "ropeld", R=[f"rope{t}"], W=["ropeT"])
        for l in range(L):
            cur[0], cur[1] = t, l
            ss = tiny[:, 0:4]
            for blk in range(4):
                ACT(xs[:], xres[:, blk, :], AF.Square, ["xres"], ["xs", "tiny"], accum=tiny[:, blk:blk + 1])
            rstd_from_ss(tiny[:, 4:8], ss, float(D), 4, ["tiny"], ["tiny"])
            for blk in range(4):
                ACT(xs[:], xres[:, blk, :], AF.Copy, ["xres", "tiny"], ["xs"], scale=tiny[:, 4 + blk:5 + blk])
                pt, pk = bank()
                ptb = pt[:].bitcast(BF16)
                for kc in range(8):
                    TR(ptb[:, kc * 128:(kc + 1) * 128], xs[:, kc * 128:(kc + 1) * 128], ["xs"], [pk])
                TT("dve", hnT[:, :, blk * 128:(blk + 1) * 128], ptb.rearrange("p (k t) -> p k t", t=128),
                   P(f"g{l}").unsqueeze(2).to_broadcast([128, 8, 128]), ALU.mult, [pk, "prm"], ["hnT"])
            chk("phase0")
            pt, pk = bank()
            for blk in range(4):
                for kc in range(8):
                    MM(pt[:, blk * 16:(blk + 1) * 16], hnT[:, kc, blk * 128:(blk + 1) * 128], wsmb[:, l, kc, :],
                       kc == 0, kc == 7, ["hnT", "wsmb"], [pk])
            TT("dve", sm[:], pt[:, 0:64].rearrange("p (b c) -> p b c", c=16),
               P(f"smb{l}").unsqueeze(1).to_broadcast([128, 4, 16]), ALU.add, [pk, "prm"], ["sm"])
            CP("dve", sp_[:, :, 0:8], sm[:, :, 0:8], ["sm"], ["sp"])
            TS("dve", sp_[:, :, 8:12], sm[:, :, 12:16], -1.0, None, ALU.mult, None, ["sm"], ["sp"])
            ACT(sp_[:], sp_[:], AF.Exp, ["sp"], ["sp"])
            ACT(sp_[:], sp_[:], AF.Ln, ["sp"], ["sp"], bias=1.0)
            TT("dve", adt[:], sp_[:, :, 0:8], lay[:, l, 8:16].unsqueeze(1).to_broadcast([128, 4, 8]), ALU.mult,
               ["sp", "lay"], ["adt"])

            chk("small")
            wax, kax = ws.use(t, l, B_AX)
            waz, kaz = ws.use(t, l, B_AZ)
            def genA(c, ch):
                HR, FR, big, tiny = HRS[ch], FRS[ch], BIGS[ch], TINYS[ch]
                bank = CURB[ch]
                xa = big[:, 0:515]
                pt, pk = bank()
                proj_fm(wax, kax, c, pt, pk)
                CP("pool", xa[:, 0:3], a_halo[:, l, c, :], ["a_halo"], ["big"])
                CP("act", xa[:, 3:515], pt[:], [pk], ["big"])
                CP("pool", a_halo[:, l, c, :], xa[:, 512:515], ["big"], ["a_halo"])
                yield ""
                cw = P(f"a_cw{l}")
                xc = FR[0]
                TS("pool", xc[:], xa[:, 3:515], cw[:, c * 4 + 3:c * 4 + 4], P(f"a_cb{l}")[:, c:c + 1], ALU.mult, ALU.add,
                   ["big", "prm"], ["fr0"])
                for j in range(3):
                    STT("dve", xc[:], xa[:, j:j + 512], cw[:, c * 4 + j:c * 4 + j + 1], xc[:], ALU.mult, ALU.add,
                        ["big", "prm", "fr0"], ["fr0"])
                pr, prk = bank()
                MM(pr[:], P(f"a_wa{l}")[:, c * 128:(c + 1) * 128], xc[:], True, True, ["prm", "fr0"], [prk])
                pi, pik = bank()
                MM(pi[:], P(f"a_wx{l}")[:, c * 128:(c + 1) * 128], xc[:], True, True, ["prm", "fr0"], [pik])
                yield ""
                r_, i_, a_, s_ = FR[1], FR[2], FR[3], FR[4]
                ACT(r_[:], pr[:], AF.Sigmoid, [prk, "prm"], ["fr1"], bias=P(f"a_ba{l}")[:, c:c + 1])
                ACT(i_[:], pi[:], AF.Sigmoid, [pik, "prm"], ["fr2"], bias=P(f"a_bx{l}")[:, c:c + 1])
                yield ""
                ACT(a_[:], r_[:], AF.Exp, ["fr1", "lay"], ["fr3"], scale=lay[:, l, c:c + 1])
                ACT(s_[:], r_[:], AF.Exp, ["fr1", "lay"], ["fr4"], scale=lay[:, l, 4 + c:5 + c])
                ACT(s_[:], s_[:], AF.Sqrt, ["fr4"], ["fr4"], bias=1.0, scale=-1.0)
                yield ""
                TT("dve", i_[:], i_[:], xc[:], ALU.mult, ["fr2", "fr0"], ["fr2"])
                TT("dve", i_[:], i_[:], s_[:], ALU.mult, ["fr2", "fr4"], ["fr2"])
                yield ""
                h_ = FR[5]
                em.op("dve", lambda e: e.tensor_tensor_scan(out=h_[:], data0=a_[:], data1=i_[:], initial=a_h[:, l, c:c + 1],
                                                            op0=ALU.mult, op1=ALU.add), ["fr3", "fr2", "a_h"], ["fr5"])
                CP("dve", a_h[:, l, c:c + 1], h_[:, 511:512], ["fr5"], ["a_h"])
                yield ""
                pz, pzk = bank()
                proj_fm(waz, kaz, c, pz, pzk)
                zs = FR[6]
                ACT(zs[:], pz[:], AF.Silu, [pzk], ["fr6"])
                yield ""
                TT("dve", yT[:, 0, c, :], h_[:], zs[:], ALU.mult, ["fr5", "fr6"], ["yT0"])
                if c == 3 and t == 0 and l == 0:
                    dd(hnT[:, 0, :], ["hnT"], True)
                    dd(xc[:], ["fr0"])
                    dd(r_[:], ["fr1"])
                    dd(a_[:], ["fr3"])
                    dd(i_[:], ["fr2"])
                    dd(h_[:], ["fr5"])
                    dd(zs[:], ["fr6"])
                    dd(yT[:, 0, c, :], ["yT0"], True)
                yield ""

            run_pairs(genA)

            ws.rel(t, l, B_AX)
            ws.rel(t, l, B_AZ)

            chk("A")
            xbcT = arenaB[:, 0:4096].rearrange("p (c t) -> p c t", t=512)
            for c8 in range(8):
                wb_, kb_ = ws.use(t, l, B_XBC0 + c8 // 4)
                xa = big[:, 0:515]
                pt, pk = bank()
                proj_fm(wb_, kb_, c8 % 4, pt, pk)
                if c8 % 4 == 3:
                    ws.rel(t, l, B_XBC0 + c8 // 4)
                CP("pool", xa[:, 0:3], b_halo[:, l, c8, :], ["b_halo"], ["big"])
                CP("act", xa[:, 3:515], pt[:], [pk], ["big"])
                CP("pool", b_halo[:, l, c8, :], xa[:, 512:515], ["big"], ["b_halo"])
                cw = P(f"b_cw{l}")
                xc = FR[c8 % 2]
                xk = f"fr{c8 % 2}"
                TS("pool", xc[:], xa[:, 3:515], cw[:, c8 * 4 + 3:c8 * 4 + 4], P(f"b_cb{l}")[:, c8:c8 + 1], ALU.mult, ALU.add,
                   ["big", "prm"], [xk])
                for j in range(3):
                    STT("dve", xc[:], xa[:, j:j + 512], cw[:, c8 * 4 + j:c8 * 4 + j + 1], xc[:], ALU.mult, ALU.add,
                        ["big", "prm", xk], [xk])
                ACT(xbcT[:, c8, :], xc[:], AF.Silu, [xk], ["arenaB"])
            wz, kz = ws.use(t, l, B_BZ)
            def genB(blk, ch):
                HR, FR, big, tiny = HRS[ch], FRS[ch], BIGS[ch], TINYS[ch]
                bank = CURB[ch]
                bs = slice(blk * 128, (blk + 1) * 128)
                pt, pk = bank()
                ptb = pt[:].bitcast(BF16)
                for c6 in range(6):
                    TR(ptb[:, c6 * 128:(c6 + 1) * 128], xbcT[:, c6, bs], ["arenaB"], [pk])
                xdt, xD, Btm = HR[0], HR[1], HR[2]
                TT("dve", xdt[:].rearrange("p (e q) -> p e q", q=64), ptb[:, 0:512].rearrange("p (e q) -> p e q", q=64),
                   sp_[:, blk, 0:8].unsqueeze(2).to_broadcast([128, 8, 64]), ALU.mult, [pk, "sp"], ["hr0"])
                TT("dve", xD[:].rearrange("p (e q) -> p e q", q=64), ptb[:, 0:512].rearrange("p (e q) -> p e q", q=64),
                   P(f"b_dsk{l}").unsqueeze(2).to_broadcast([128, 8, 64]), ALU.mult, [pk, "prm"], ["hr1"])
                CP("act", Btm[:, 0:256], ptb[:, 512:768], [pk], ["hr2"])
                yield ""
                pm, pmk = bank()
                MM(pm[:, 0:8], Vm[:], adt[:, blk, :], True, True, ["Vm", "adt"], [pmk])
                MM(pm[:, 8:16], ones[:], adt[:, blk, :], True, True, ["ones", "adt"], [pmk])
                lhsE = big[:].rearrange("p (e s) -> p e s", s=128)
                TT("dve", lhsE, Um[:].unsqueeze(1).to_broadcast([128, 8, 128]),
                   adt[:, blk, :].unsqueeze(2).to_broadcast([128, 8, 128]), ALU.mult, ["Um", "adt"], ["big"])
                yield ""
                acs = tiny[:, 16:24]
                atot = tiny[:, 24:32]
                CP("dve", tiny[:, 16:32], pm[:, 0:16], [pmk], ["tiny"])
                ea, dte, cd = tiny[:, 32:40], tiny[:, 40:48], tiny[:, 48:56]
                ACT(ea, acs, AF.Exp, ["tiny"], ["tiny"])
                TT("dve", dte, atot, acs, ALU.subtract, ["tiny"], ["tiny"])
                ACT(dte, dte, AF.Exp, ["tiny"], ["tiny"])
                ACT(cd, atot, AF.Exp, ["tiny"], ["tiny"])
                yield ""
                pd0, pd0k = bank()
                pd1, pd1k = bank()
                for e8 in range(8):
                    pd, pdk = (pd0, pd0k) if e8 < 4 else (pd1, pd1k)
                    MM(pd[:, (e8 % 4) * 128:(e8 % 4 + 1) * 128], lhsE[:, e8, :], Vm[:], True, True, ["big", "Vm"], [pdk])
                yield ""
                Lm = HR[3], HR[4]
                ACT(Lm[0][:], pd0[:], AF.Exp, [pd0k], ["hr3"])
                ACT(Lm[1][:], pd1[:], AF.Exp, [pd1k], ["hr4"])
                pcb, pcbk = bank()
                for g in range(2):
                    MM(pcb[:, g * 128:(g + 1) * 128], xbcT[:, 4 + g, bs], xbcT[:, 6 + g, bs], True, True, ["arenaB"], [pcbk])
                yield ""
                cbm = HR[5]
                TT("dve", cbm[:, 0:256].rearrange("p (g s) -> p g s", s=128), pcb[:, 0:256].rearrange("p (g s) -> p g s", s=128),
                   Vb[:].unsqueeze(1).to_broadcast([128, 2, 128]), ALU.mult, [pcbk, "Vb"], ["hr5"])
                xdd = HR[7]
                TT("dve", xdd[:].rearrange("p (e q) -> p e q", q=64), xdt[:].rearrange("p (e q) -> p e q", q=64),
                   dte.unsqueeze(2).to_broadcast([128, 8, 64]), ALU.mult, ["hr0", "tiny"], ["hr7"])
                yield ""
                for g in range(2):
                    TT("dve", Lm[g][:].rearrange("p (e s) -> p e s", s=128), Lm[g][:].rearrange("p (e s) -> p e s", s=128),
                       cbm[:, g * 128:(g + 1) * 128].unsqueeze(1).to_broadcast([128, 4, 128]), ALU.mult,
                       [f"hr{3 + g}", "hr5"], [f"hr{3 + g}"])
                yield ""
                py, pyk = bank()
                for e8 in range(8):
                    MM(py[:, e8 * 64:(e8 + 1) * 64], Lm[e8 // 4][:, (e8 % 4) * 128:(e8 % 4 + 1) * 128], xdt[:, e8 * 64:(e8 + 1) * 64],
                       True, True, [f"hr{3 + e8 // 4}", "hr0"], [pyk])
                pz, pzk = bank()
                proj_tm(wz, kz, blk, pz, pzk)
                zs = FR[3]
                ACT(zs[:], pz[:], AF.Silu, [pzk], ["fr3"])
                yield "R"
                pyo, pyok = bank()
                for g in range(2):
                    MM(pyo[:, g * 256:(g + 1) * 256], xbcT[:, 6 + g, bs], b_stb[:, l, g * 256:(g + 1) * 256], True, True,
                       ["arenaB", "b_stb"], [pyok])
                t1 = FR[2]
                TT("dve", t1[:].rearrange("p (e q) -> p e q", q=64), pyo[:].rearrange("p (e q) -> p e q", q=64),
                   ea.unsqueeze(2).to_broadcast([128, 8, 64]), ALU.mult, [pyok, "tiny"], ["fr2"])
                TT("dve", t1[:], t1[:], py[:], ALU.add, ["fr2", pyk], ["fr2"])
                pst, pstk = bank()
                for g in range(2):
                    MM(pst[:, g * 256:(g + 1) * 256], Btm[:, g * 128:(g + 1) * 128], xdd[:, g * 256:(g + 1) * 256], True, True,
                       ["hr2", "hr7"], [pstk])
                TT("dve", b_st[:, l, :].rearrange("p (e q) -> p e q", q=64), b_st[:, l, :].rearrange("p (e q) -> p e q", q=64),
                   cd.unsqueeze(2).to_broadcast([128, 8, 64]), ALU.mult, ["b_st", "tiny"], ["b_st"])
                TT("dve", b_st[:, l, :], b_st[:, l, :], pst[:], ALU.add, ["b_st", pstk], ["b_st"])
                CP("act", b_stb[:, l, :], b_st[:, l, :], ["b_st"], ["b_stb"])
                yield "U"
                TT("dve", t1[:], t1[:], xD[:], ALU.add, ["fr2", "hr1"], ["fr2"])
                yield ""
                TT("dve", t1[:], t1[:], zs[:], ALU.mult, ["fr2", "fr3"], ["fr2"])
                ACT(zs[:], t1[:], AF.Square, ["fr2"], ["fr3", "tiny"], accum=tiny[:, 56:57])
                yield ""
                rstd_from_ss(tiny[:, 57:58], tiny[:, 56:57], 512.0, 1, ["tiny"], ["tiny"])
                yn = HR[6]
                ACT(yn[:], t1[:], AF.Copy, ["fr2", "tiny"], ["hr6"], scale=tiny[:, 57:58])
                yield ""
                pt2, pt2k = bank()
                pt2b = pt2[:].bitcast(BF16)
                for c in range(4):
                    TR(pt2b[:, c * 128:(c + 1) * 128], yn[:, c * 128:(c + 1) * 128], ["hr6"], [pt2k])
                TT("dve", yT[:, 1, :, bs], pt2b[:, 0:512].rearrange("p (c t) -> p c t", t=128),
                   P(f"b_g{l}").unsqueeze(2).to_broadcast([128, 4, 128]), ALU.mult, [pt2k, "prm"], ["yT1"])
                yield ""

            qT = arena[:, 0:2048].rearrange("p (h t) -> p h t", t=512)
            kT = arena[:, 2048:4096].rearrange("p (h t) -> p h t", t=512)
            zsT = arena[:, 4096:6144].rearrange("p (h t) -> p h t", t=512)
            WB = {}

            def genCprep():
                bank = PBANK
                wq, kq = ws.use(t, l, B_CQ)
                wk_, kk = ws.use(t, l, B_CK)
                WB["ck"] = (wk_, kk)
                for h in range(4):
                    pt, pk = bank()
                    proj_fm(wq, kq, h, pt, pk)
                    CP("act", qT[:, h, :], pt[:], [pk], ["arena"])
                    yield ""
                    pt, pk = bank()
                    proj_fm(wk_, kk, h, pt, pk)
                    em.op("act", lambda e: e.mul(kT[:, h, :], pt[:], 128.0 ** -0.5), [pk], ["arena"])
                    yield ""
                ws.rel(t, l, B_CQ)
                wzc, kzc = ws.use(t, l, B_CZ)
                for c in range(4):
                    pt, pk = bank()
                    proj_fm(wzc, kzc, c, pt, pk)
                    ACT(FRP[:], pt[:], AF.Silu, [pk], ["frp"])
                    TS("pool", zsT[:, c, :], FRP[:], P(f"c_g{l}")[:, c:c + 1], None, ALU.mult, None, ["frp", "prm"], ["arena"])
                    yield ""
                ws.rel(t, l, B_CZ)

            run_pairs(genB, extra=genCprep())
            ws.rel(t, l, B_BZ)

            chk("B")
            wk_, kk = WB["ck"]
            wv, kv = ws.use(t, l, B_CV)
            wo, ko = ws.use(t, l, B_CO)
            def genC(blk, ch):
                HR, FR, big, tiny = HRS[ch], FRS[ch], BIGS[ch], TINYS[ch]
                bank = CURB[ch]
                bs = slice(blk * 128, (blk + 1) * 128)
                kTM, vTM, so = HR[0], HR[1], HR[2]
                pt, pk = bank()
                proj_tm(wk_, kk, blk, pt, pk)
                em.op("act", lambda e: e.mul(kTM[:], pt[:], 128.0 ** -0.5), [pk], ["hr0"])
                yield ""
                pt, pk = bank()
                proj_tm(wv, kv, blk, pt, pk)
                CP("act", vTM[:], pt[:], [pk], ["hr1"])
                yield ""
                pt, pk = bank()
                proj_tm(wo, ko, blk, pt, pk)
                ACT(so[:], pt[:], AF.Sigmoid, [pk], ["hr2"])
                yield ""
                lnf = sp_[:, blk, 8:12]
                logi = sm[:, blk, 8:12]
                pm, pmk = bank()
                MM(pm[:, 0:4], Vm[:], lnf, True, True, ["Vm", "sp"], [pmk])
                MM(pm[:, 4:8], ones[:], lnf, True, True, ["ones", "sp"], [pmk])
                lhsC = big[:, 0:512].rearrange("p (h s) -> p h s", s=128)
                STT("dve", lhsC, lnf.unsqueeze(2).to_broadcast([128, 4, 128]), -1.0,
                    Um[:].unsqueeze(1).to_broadcast([128, 4, 128]), ALU.mult, ALU.mult, ["Um", "sp"], ["big"])
                yield ""
                Fn, Ftn = tiny[:, 16:20], tiny[:, 20:24]
                CP("dve", tiny[:, 16:24], pm[:, 0:8], [pmk], ["tiny"])
                eF, eFt, wend = tiny[:, 24:28], tiny[:, 28:32], tiny[:, 32:36]
                ACT(eF, Fn, AF.Exp, ["tiny"], ["tiny"], scale=-1.0)
                ACT(eFt, Ftn, AF.Exp, ["tiny"], ["tiny"], scale=-1.0)
                TT("dve", wend, Fn, Ftn, ALU.subtract, ["tiny"], ["tiny"])
                TT("dve", wend, wend, logi, ALU.add, ["tiny", "sm"], ["tiny"])
                ACT(wend, wend, AF.Exp, ["tiny"], ["tiny"])
                yield ""
                pdf, pdfk = bank()
                for h in range(4):
                    MM(pdf[:, h * 128:(h + 1) * 128], lhsC[:, h, :], Vm[:], True, True, ["big", "Vm"], [pdfk])
                psc, psck = bank()
                for h in range(4):
                    MM(psc[:, h * 128:(h + 1) * 128], kT[:, h, bs], qT[:, h, bs], True, True, ["arena"], [psck])
                yield ""
                Dt = HR[3]
                for h in range(4):
                    ACT(Dt[:, h * 128:(h + 1) * 128], pdf[:, h * 128:(h + 1) * 128], AF.Exp, [pdfk, "sm"], ["hr3"],
                        bias=logi[:, h:h + 1])
                kw = HR[6]
                TT("dve", kw[:].rearrange("p (h s) -> p h s", s=128), kTM[:].rearrange("p (h s) -> p h s", s=128),
                   wend.unsqueeze(2).to_broadcast([128, 4, 128]), ALU.mult, ["hr0", "tiny"], ["hr6"])
                yield ""
                TT("dve", Dt[:].rearrange("p (h s) -> p h s", s=128), Dt[:].rearrange("p (h s) -> p h s", s=128),
                   Vb[:].unsqueeze(1).to_broadcast([128, 4, 128]), ALU.mult, ["hr3", "Vb"], ["hr3"])
                yield ""
                Pt = HR[4]
                TT("dve", Pt[:], psc[:], Dt[:], ALU.mult, [psck, "hr3"], ["hr4"])
                yield ""
                pnum, pnumk = bank()
                for h in range(4):
                    MM(pnum[:, h * 128:(h + 1) * 128], Pt[:, h * 128:(h + 1) * 128], vTM[:, h * 128:(h + 1) * 128], True, True,
                       ["hr4", "hr1"], [pnumk])
                yield "R"
                pin, pink = bank()
                for h in range(4):
                    MM(pin[:, h * 128:(h + 1) * 128], qT[:, h, bs], c_stb[:, l, h, 0:128], True, True, ["arena", "c_stb"], [pink])
                pden, pdenk = bank()
                for h in range(4):
                    MM(pden[:, 2 * h:2 * h + 2], Pt[:, h * 128:(h + 1) * 128], onesb[:], True, True, ["hr4", "onesb"], [pdenk])
                for h in range(4):
                    MM(pden[:, 8 + 2 * h:10 + 2 * h], qT[:, h, bs], c_stb[:, l, h, 128:130], True, True,
                       ["arena", "c_stb"], [pdenk])
                t1 = FR[1]
                TT("dve", t1[:].rearrange("p (h s) -> p h s", s=128), pin[:].rearrange("p (h s) -> p h s", s=128),
                   eF.unsqueeze(2).to_broadcast([128, 4, 128]), ALU.mult, [pink, "tiny"], ["fr1"])
                TT("dve", t1[:], t1[:], pnum[:], ALU.add, ["fr1", pnumk], ["fr1"])
                den = tiny[:, 36:52]
                CP("dve", den, pden[:, 0:16], [pdenk], ["tiny"])
                pS, pSk = bank()
                for h in range(4):
                    MM(pS[:, h * 128:(h + 1) * 128], kw[:, h * 128:(h + 1) * 128], vTM[:, h * 128:(h + 1) * 128], True, True,
                       ["hr6", "hr1"], [pSk])
                pn, pnk = bank()
                for h in range(4):
                    MM(pn[:, 2 * h:2 * h + 2], kw[:, h * 128:(h + 1) * 128], onesb[:], True, True, ["hr6", "onesb"], [pnk])
                TT("dve", c_st[:, l], c_st[:, l], eFt.unsqueeze(2).to_broadcast([128, 4, 130]), ALU.mult,
                   ["c_st", "tiny"], ["c_st"])
                TT("dve", c_st[:, l, :, 0:128], c_st[:, l, :, 0:128], pS[:].rearrange("p (h s) -> p h s", s=128), ALU.add,
                   ["c_st", pSk], ["c_st"])
                TT("dve", c_st[:, l, :, 128:130], c_st[:, l, :, 128:130], pn[:, 0:8].rearrange("p (h two) -> p h two", two=2),
                   ALU.add, ["c_st", pnk], ["c_st"])
                CP("act", c_stb[:, l], c_st[:, l], ["c_st"], ["c_stb"])
                yield "U"
                dv = den.rearrange("p (a h two) -> p a h two", a=2, two=2)
                dn, rec, r2 = tiny[:, 52:56], tiny[:, 56:60], tiny[:, 60:64]
                TT("dve", dn, dv[:, 1, :, 0], eF, ALU.mult, ["tiny"], ["tiny"])
                TT("dve", dn, dn, dv[:, 0, :, 0], ALU.add, ["tiny"], ["tiny"])
                TS("dve", rec, dn, -1.0, None, ALU.mult, None, ["tiny"], ["tiny"])
                TT("dve", rec, rec, dn, ALU.max, ["tiny"], ["tiny"])
                TS("dve", rec, rec, 1.0, None, ALU.max, None, ["tiny"], ["tiny"])
                em.op("dve", lambda e: e.reciprocal(out=rec, in_=rec), ["tiny"], ["tiny"])
                yield ""
                TT("dve", t1[:], t1[:], so[:], ALU.mult, ["fr1", "hr2"], ["fr1"])
                yield ""
                ssq = tiny[:, 64:68]
                for h in range(4):
                    ACT(FR[2][:, 0:128], t1[:, h * 128:(h + 1) * 128], AF.Square, ["fr1"], ["fr2", "tiny"], accum=ssq[:, h:h + 1])
                yield ""
                TT("dve", r2, rec, rec, ALU.mult, ["tiny"], ["tiny"])
                TT("dve", r2, r2, ssq, ALU.mult, ["tiny"], ["tiny"])
                rstd_from_ss(r2, r2, 128.0, 4, ["tiny"], ["tiny"])
                TT("dve", r2, r2, rec, ALU.mult, ["tiny"], ["tiny"])
                yield ""
                hn = HR[5]
                TT("dve", hn[:].rearrange("p (h s) -> p h s", s=128), t1[:].rearrange("p (h s) -> p h s", s=128),
                   r2.unsqueeze(2).to_broadcast([128, 4, 128]), ALU.mult, ["fr1", "tiny"], ["hr5"])
                pt2, pt2k = bank()
                pt2b = pt2[:].bitcast(BF16)
                for h in range(4):
                    TR(pt2b[:, h * 128:(h + 1) * 128], hn[:, h * 128:(h + 1) * 128], ["hr5"], [pt2k])
                TT("dve", yT[:, 2, :, bs], pt2b[:, 0:512].rearrange("p (c t) -> p c t", t=128), zsT[:, :, bs], ALU.mult,
                   [pt2k, "arena"], ["yT2"])
                yield ""

            zsD = arenaB[:, 0:2048].rearrange("p (h t) -> p h t", t=512)

            def genDprep():
                bank = PBANK
                wdz, kdz = ws.use(t, l, B_DZ)
                for c in range(4):
                    pt, pk = bank()
                    proj_fm(wdz, kdz, c, pt, pk)
                    ACT(FRP[:], pt[:], AF.Silu, [pk], ["frp"])
                    TS("pool", zsD[:, c, :], FRP[:], P(f"d_g{l}")[:, c:c + 1], None, ALU.mult, None, ["frp", "prm"], ["arenaB"])
                    yield ""
                ws.rel(t, l, B_DZ)

            run_pairs(genC, extra=genDprep())
            ws.rel(t, l, B_CK)
            ws.rel(t, l, B_CV)
            ws.rel(t, l, B_CO)

            chk("C")
            wqk, kqk = ws.use(t, l, B_DQK)
            wdv, kdv = ws.use(t, l, B_DV)
            def genD(blk, ch):
                HR, FR, big, tiny = HRS[ch], FRS[ch], BIGS[ch], TINYS[ch]
                bank = CURB[ch]
                bs = slice(blk * 128, (blk + 1) * 128)
                vD = HR[0]
                pt, pk = bank()
                proj_tm(wdv, kdv, blk, pt, pk)
                CP("act", vD[:], pt[:], [pk], ["hr0"])
                yield ""
                pq, pqk = bank()
                proj_tm(wqk, kqk, blk, pq, pqk)
                yield ""
                qv = pq[:].rearrange("p (h two i) -> p h two i", two=2, i=32)
                cosb = ropeT[:, blk, 0:32].unsqueeze(1).to_broadcast([128, 8, 32])
                sinb = ropeT[:, blk, 32:64].unsqueeze(1).to_broadcast([128, 8, 32])
                qr = FR[1][:].rearrange("p (h two i) -> p h two i", two=2, i=32)
                ta = FR[2][:, 0:256].rearrange("p (h i) -> p h i", i=32)
                tb = FR[2][:, 256:512].rearrange("p (h i) -> p h i", i=32)
                TT("dve", qr[:, :, 0, :], qv[:, :, 0, :], cosb, ALU.mult, [pqk, "ropeT"], ["fr1"])
                TT("dve", ta, qv[:, :, 1, :], sinb, ALU.mult, [pqk, "ropeT"], ["fr2"])
                yield ""
                TT("dve", qr[:, :, 0, :], qr[:, :, 0, :], ta, ALU.subtract, ["fr1", "fr2"], ["fr1"])
                TT("dve", qr[:, :, 1, :], qv[:, :, 0, :], sinb, ALU.mult, [pqk, "ropeT"], ["fr1"])
                TT("dve", tb, qv[:, :, 1, :], cosb, ALU.mult, [pqk, "ropeT"], ["fr2"])
                yield ""
                TT("dve", qr[:, :, 1, :], qr[:, :, 1, :], tb, ALU.add, ["fr1", "fr2"], ["fr1"])
                yield ""
                qrs = HR[1]
                TT("dve", qrs[:].rearrange("p (h d) -> p h d", d=64), FR[1][:].rearrange("p (h d) -> p h d", d=64),
                   cst[:, 1:9].unsqueeze(2).to_broadcast([128, 8, 64]), ALU.mult, ["fr1", "cst"], ["hr1"])
                yield ""
                pt2, pt2k = bank()
                pt2b = pt2[:].bitcast(BF16)
                for c in range(4):
                    TR(pt2b[:, c * 128:(c + 1) * 128], qrs[:, c * 128:(c + 1) * 128], ["hr1"], [pt2k])
                qkT = HR[2]
                CP("act", qkT[:], pt2b[:, 0:512], [pt2k], ["hr2"])
                yield ""
                pscs = [bank(), bank()]
                for h in range(4):
                    a_, j_ = h % 2, h // 2
                    pr_ = a_ * 64
                    MM(pscs[a_][0][:, j_ * 128:(j_ + 1) * 128], qkT[pr_:pr_ + 64, (2 + j_) * 128:(3 + j_) * 128],
                       qkT[pr_:pr_ + 64, j_ * 128:(j_ + 1) * 128], True, True, ["hr2"], [pscs[a_][1]])
                yield ""
                Pt = HR[3]
                for a_ in range(2):
                    TT("dve", Pt[:, a_ * 256:(a_ + 1) * 256].rearrange("p (j s) -> p j s", s=128),
                       pscs[a_][0][:, 0:256].rearrange("p (j s) -> p j s", s=128),
                       Vb[:].unsqueeze(1).to_broadcast([128, 2, 128]), ALU.mult, [pscs[a_][1], "Vb"], ["hr3"])
                yield "R"
                pys = [bank(), bank()]
                for h in range(4):
                    a_, j_ = h % 2, h // 2
                    pr_ = a_ * 64
                    MM(pys[a_][0][:, j_ * 128:(j_ + 1) * 128], Pt[:, (a_ * 2 + j_) * 128:(a_ * 2 + j_ + 1) * 128],
                       vD[:, h * 128:(h + 1) * 128], True, False, ["hr3", "hr0"], [pys[a_][1]])
                    MM(pys[a_][0][:, j_ * 128:(j_ + 1) * 128], qkT[pr_:pr_ + 64, j_ * 128:(j_ + 1) * 128],
                       d_stb[pr_:pr_ + 64, l, j_, :], False, True, ["hr2", "d_stb"], [pys[a_][1]])
                pR, pRk = bank()
                for h in range(4):
                    pr_ = (h % 2) * 64
                    MM(pR[pr_:pr_ + 64, (h // 2) * 128:(h // 2 + 1) * 128], qrs[:, (4 + h) * 64:(5 + h) * 64],
                       vD[:, h * 128:(h + 1) * 128], True, True, ["hr1", "hr0"], [pRk])
                TT("dve", d_st[:, l], d_st[:, l], pR[:, 0:256].rearrange("p (j e) -> p j e", e=128), ALU.add,
                   ["d_st", pRk], ["d_st"])
                for j in range(2):
                    TS("dve", d_st[:, l, j, :], d_st[:, l, j, :], cst[:, 9 + j:10 + j], None, ALU.mult, None,
                       ["d_st", "cst"], ["d_st"])
                CP("act", d_stb[:, l], d_st[:, l], ["d_st"], ["d_stb"])
                yield "U"
                ssq = tiny[:, 16:20]
                for h in range(4):
                    a_, j_ = h % 2, h // 2
                    ACT(FR[3][:, 0:128], pys[a_][0][:, j_ * 128:(j_ + 1) * 128], AF.Square, [pys[a_][1]], ["fr3", "tiny"],
                        accum=ssq[:, h:h + 1])
                yield ""
                rstd_from_ss(tiny[:, 20:24], ssq, 128.0, 4, ["tiny"], ["tiny"])
                yield ""
                hn = HR[4]
                for h in range(4):
                    a_, j_ = h % 2, h // 2
                    TS("dve", hn[:, h * 128:(h + 1) * 128], pys[a_][0][:, j_ * 128:(j_ + 1) * 128], tiny[:, 20 + h:21 + h], None,
                       ALU.mult, None, [pys[a_][1], "tiny"], ["hr4"])
                yield ""
                pt3, pt3k = bank()
                pt3b = pt3[:].bitcast(BF16)
                for h in range(4):
                    TR(pt3b[:, h * 128:(h + 1) * 128], hn[:, h * 128:(h + 1) * 128], ["hr4"], [pt3k])
                TT("dve", yT[:, 3, :, bs], pt3b[:, 0:512].rearrange("p (c t) -> p c t", t=128), zsD[:, :, bs], ALU.mult,
                   [pt3k, "arenaB"], ["yT3"])
                yield ""

            run_pairs(genD)
            ws.rel(t, l, B_DQK)
            ws.rel(t, l, B_DV)

            chk("D")
            if dbg:
                em.dma("sp", dbg_d.ap()[t * L + l], yT[:].rearrange("p a b c -> p (a b c)"), "dbg",
                       R=["yT0", "yT1", "yT2", "yT3"])

            if l == L - 1 and t + 1 < NT:
                rope_gen(t + 1, FR[3:7], ["fr3", "fr4", "fr5", "fr6"])
            mT = arena[:, 0:4096].rearrange("p (k t) -> p k t", t=512)
            for jb in range(2):
                for n in range(4):
                    wg, kg = ws.use(t, l, blk_gate(n, jb))
                    wbb, kbb = ws.use(t, l, blk_wb(n // 2, jb))
                    for j in range(4):
                        pg, pgk = bank()
                        proj_fm(wg, kg, j, pg, pgk)
                        pu, puk = bank()
                        for kc in range(4):
                            o_ = (n % 2) * 2048 + kc * 512 + j * 128
                            MM(pu[:], wbb[:, o_:o_ + 128], yT[:, n, kc, :], kc == 0, kc == 3, [kbb, f"yT{n}"], [puk])
                        sg = FR[(n * 4 + j) % 2]
                        sgk = f"fr{(n * 4 + j) % 2}"
                        ACT(sg[:], pg[:], AF.Sigmoid, [pgk], [sgk])
                        if n == 0:
                            TT("dve", FR2[j][:], sg[:], pu[:], ALU.mult, [sgk, puk], [f"fr{j}_b"])
                        else:
                            TT("dve", sg[:], sg[:], pu[:], ALU.mult, [sgk, puk], [sgk])
                            if n < 3:
                                TT("pool", FR2[j][:], FR2[j][:], sg[:], ALU.add, [f"fr{j}_b", sgk], [f"fr{j}_b"])
                            else:
                                TT("pool", mT[:, jb * 4 + j, :], FR2[j][:], sg[:], ALU.add, [f"fr{j}_b", sgk], ["arena"])
                    ws.rel(t, l, blk_gate(n, jb))
                    if n % 2 == 1:
                        ws.rel(t, l, blk_wb(n // 2, jb))
            chk("merge")
            wo0, ko0 = ws.use(t, l, B_WOUT)
            wo1, ko1 = ws.use(t, l, B_WOUT + 1)
            for blk in range(4):
                for hf in range(2):
                    wt_, wk2 = (wo0, ko0) if hf == 0 else (wo1, ko1)
                    po, pok = bank()
                    for kc in range(8):
                        MM(po[:], mT[:, kc, blk * 128:(blk + 1) * 128], wt_[:, kc * 512:(kc + 1) * 512], kc == 0, kc == 7,
                           ["arena", wk2], [pok])
                    TT("dve", xres[:, blk, hf * 512:(hf + 1) * 512], xres[:, blk, hf * 512:(hf + 1) * 512], po[:], ALU.add,
                       ["xres", pok], ["xres"])
            ws.rel(t, l, B_WOUT)
            ws.rel(t, l, B_WOUT + 1)
        for blk in range(4):
            ACT(xs[:], xres[:, blk, :], AF.Square, ["xres"], ["xs", "tiny"], accum=tiny[:, blk:blk + 1])
        rstd_from_ss(tiny[:, 4:8], tiny[:, 0:4], float(D), 4, ["tiny"], ["tiny"])
        for blk in range(4):
            STT("dve", xres[:, blk, :], xres[:, blk, :], tiny[:, 4 + blk:5 + blk], P("gf"), ALU.mult, ALU.mult,
                ["xres", "tiny", "prm"], ["xres"])
        em.dma("sp", y_d.ap()[t * 512:(t + 1) * 512, :].rearrange("(b p) d -> p b d", p=128), xres[:], "yst", R=["xres"])
    em.finish("sp")
    print("instructions", em.ninst, "waits", em.nwait)
    return nc


_CACHE = {}


def run(inp, NT, dbg=False, ncores=8, stop=None):
    x = np.asarray(inp["x"], np.float32)
    B = x.shape[0]
    prm = pack_params(inp)
    wst = pack_weights(inp)
    key = (NT, dbg, stop)
    if key not in _CACHE:
        _CACHE[key] = build(NT, dbg, stop)
    nc = _CACHE[key]
    in_maps = []
    for c in range(ncores):
        b = c % B
        in_maps.append({"x": np.ascontiguousarray(x[b, :NT * 512]), "wst": wst, "prm": prm})
    res = run_bass_kernel_spmd(nc, in_maps, core_ids=list(range(ncores)))
    return res


def kernel(**inputs):
    inp = {k: np.asarray(v) for k, v in inputs.items()}
    NT = inp["x"].shape[1] // 512
    res = run(inp, NT)
    B = inp["x"].shape[0]
    return np.stack([res.results[b]["y"] for b in range(B)], axis=0).astype(np.float32)
```

```python
import math
import numpy as np
import concourse.bass as bass
import concourse.mybir as mybir
from concourse.bass_utils import run_bass_kernel_spmd

F32 = mybir.dt.float32
BF16 = mybir.dt.bfloat16
I32 = mybir.dt.int32
AF = mybir.ActivationFunctionType
ALU = mybir.AluOpType

L = 2
D = 1024
EPS = 1e-6
NBLK = 27
NSLOT = 6
W_IN = 10768
C_AX, C_AZ, C_BX, C_BZ, C_BDT = 0, 512, 1024, 2048, 2560
C_CQ, C_CK, C_CV, C_CO, C_CZ, C_CI, C_CF = 2568, 3080, 3592, 4104, 4616, 5128, 5132
C_DQ, C_DK, C_DV, C_DZ, C_G = 5136, 5392, 5648, 6160, 6672
BLK_COLS = [C_AX, C_AZ, C_BX, C_BX + 512, C_BZ, C_CQ, C_CK, C_CZ, C_CV, C_CO, C_DZ, C_DQ, C_DV]
(B_AX, B_AZ, B_XBC0, B_XBC1, B_BZ, B_CQ, B_CK, B_CZ, B_CV, B_CO, B_DZ, B_DQK, B_DV) = range(13)
B_WOUT = 25


def blk_gate(n, jb):
    return 13 + jb * 6 + [0, 2, 3, 5][n]


def blk_wb(pair, jb):
    return 13 + jb * 6 + [1, 4][pair]


def prm_layout():
    off = {}
    p = 0

    def add(name, n):
        nonlocal p
        off[name] = (p, n)
        p += n

    for l in range(L):
        add(f"g{l}", 8)
        add(f"a_cw{l}", 16)
        add(f"a_cb{l}", 4)
        add(f"a_ba{l}", 4)
        add(f"a_bx{l}", 4)
        add(f"a_lam{l}", 4)
        add(f"a_wa{l}", 512)
        add(f"a_wx{l}", 512)
        add(f"b_cw{l}", 32)
        add(f"b_cb{l}", 8)
        add(f"smb{l}", 16)
        add(f"b_alog{l}", 8)
        add(f"b_dsk{l}", 8)
        add(f"b_g{l}", 4)
        add(f"c_g{l}", 4)
        add(f"d_g{l}", 4)
        add(f"wsm{l}", 128)
    add("gf", 1024)
    return off, p


PRM_OFF, NPRM = prm_layout()


def pack_params(inp):
    prm = np.zeros((128, NPRM), np.float32)

    def put(name, arr):
        o, n = PRM_OFF[name]
        prm[:, o:o + n] = np.asarray(arr, np.float32).reshape(128, n)

    def fm(v, nch):
        return np.asarray(v).reshape(nch, 128).T

    def bc(v):
        return np.broadcast_to(np.asarray(v)[None, :], (128, len(v)))

    for l in range(L):
        put(f"g{l}", fm(inp["norm_g"][l], 8))
        put(f"a_cw{l}", inp["a_conv_w"][l].reshape(4, 4, 128).transpose(2, 1, 0))
        put(f"a_cb{l}", fm(inp["a_conv_b"][l], 4))
        put(f"a_ba{l}", fm(inp["a_gate_a_b"][l], 4))
        put(f"a_bx{l}", fm(inp["a_gate_x_b"][l], 4))
        put(f"a_lam{l}", fm(inp["a_lambda"][l], 4))
        for nm, src in ((f"a_wa{l}", inp["a_gate_a_w"][l]), (f"a_wx{l}", inp["a_gate_x_w"][l])):
            m = np.zeros((128, 4, 128), np.float32)
            for c in range(4):
                m[0:64, c, 0:64] = src[2 * c]
                m[64:128, c, 64:128] = src[2 * c + 1]
            put(nm, m)
        put(f"b_cw{l}", inp["b_conv_w"][l].reshape(4, 8, 128).transpose(2, 1, 0))
        put(f"b_cb{l}", fm(inp["b_conv_b"][l], 8))
        put(f"smb{l}", bc(np.concatenate([inp["b_dt_bias"][l], inp["c_i_bias"][l], inp["c_f_bias"][l]])))
        put(f"b_alog{l}", bc(inp["b_a_log"][l]))
        put(f"b_dsk{l}", bc(inp["b_d_skip"][l]))
        put(f"b_g{l}", fm(inp["b_norm_g"][l], 4))
        put(f"c_g{l}", fm(inp["c_norm_g"][l], 4))
        put(f"d_g{l}", fm(inp["d_norm_g"][l], 4))
        cols = list(range(C_BDT, C_BDT + 8)) + list(range(C_CI, C_CI + 8))
        put(f"wsm{l}", inp["w_in"][l][:, cols].reshape(8, 128, 16).transpose(1, 0, 2))
    put("gf", bc(inp["final_norm_g"]))
    return prm


def pack_weights(inp):
    wst = np.empty((L, NBLK, 128, 4096), np.float32)
    for l in range(L):
        w_in = inp["w_in"][l]

        def inblk(c0):
            return w_in[:, c0:c0 + 512].reshape(8, 128, 512).transpose(1, 0, 2).reshape(128, 4096)

        for b, c0 in enumerate(BLK_COLS):
            wst[l, b] = inblk(c0)
        for jb in range(2):
            for n in range(4):
                wst[l, blk_gate(n, jb)] = inblk(C_G + n * 1024 + jb * 512)
            for pair in range(2):
                m = np.stack([inp["w_branch"][l, 2 * pair + i][:, jb * 512:(jb + 1) * 512]
                              .reshape(4, 128, 512).transpose(1, 0, 2) for i in range(2)], axis=1)
                wst[l, blk_wb(pair, jb)] = m.reshape(128, 4096)
        for hf in range(2):
            wst[l, B_WOUT + hf] = inp["w_out"][l][:, hf * 512:(hf + 1) * 512] \
                .reshape(8, 128, 512).transpose(1, 0, 2).reshape(128, 4096)
    return wst.reshape(L * NBLK * 256, 2048)


import re
_SCR = re.compile(r"(hr\d|fr\d|big|tiny)")


class Em:
    ENG = ("pe", "act", "dve", "pool", "sp")

    def __init__(self, nc):
        self.nc = nc
        self.e = {"pe": nc.tensor, "act": nc.scalar, "dve": nc.vector, "pool": nc.gpsimd, "sp": nc.sync}
        self.sem = {k: nc.alloc_semaphore("sem_" + k) for k in self.ENG}
        self.cnt = {k: 0 for k in self.ENG}
        self.dsem = {}
        self.dcnt = {}
        self.seen = {k: {} for k in self.ENG}
        self.last_w = {}
        self.readers = {}
        self.gen = {}
        self.sfx = ""
        self.ninst = 0
        self.nwait = 0

    def _canon(self, k):
        if self.sfx and _SCR.fullmatch(k):
            return k + self.sfx
        if "#" in k:
            base, g = k.split("#")
            assert self.gen.get(base) == int(g), f"stale psum handle {k} (cur {self.gen.get(base)})"
            return base
        return k

    def _sem_of(self, s):
        return self.sem[s] if s in self.sem else self.dsem[s]

    def _need(self, eng, deps):
        best = {}
        for d in deps:
            if d is None:
                continue
            s, v, pe, raw = d
            if pe == eng and (eng == "pe" or not raw):
                continue
            if self.seen[eng].get(s, 0) >= v:
                continue
            if best.get(s, 0) < v:
                best[s] = v
        for s, v in best.items():
            self.e[eng].wait_ge(self._sem_of(s), v)
            self.seen[eng][s] = v
            self.nwait += 1

    def _deps(self, R, W):
        deps = []
        for k in R:
            d = self.last_w.get(k)
            if d is not None:
                deps.append(d + (True,))
        for k in W:
            d = self.last_w.get(k)
            if d is not None:
                deps.append(d + (False,))
            deps.extend(r + (False,) for r in self.readers.get(k, ()))
        return deps

    def _mark(self, tag, R, W):
        for k in W:
            self.last_w[k] = tag
            self.readers[k] = []
        for k in R:
            if k not in W:
                self.readers.setdefault(k, []).append(tag)

    def op(self, eng, fn, R=(), W=()):
        R = [self._canon(k) for k in R]
        W = [self._canon(k) for k in W]
        W = W + [k for k in R if k.startswith("ps") and k not in W]
        R = [k for k in R if not k.startswith("ps")]
        self._need(eng, self._deps(R, W))
        ins = fn(self.e[eng])
        self.cnt[eng] += 1
        ins.then_inc(self.sem[eng], 1)
        self._mark((eng, self.cnt[eng], eng), R, W)
        self.ninst += 1

    def dma(self, q, out, in_, stream, R=(), W=(), **kw):
        R = [self._canon(k) for k in R]
        W = [self._canon(k) for k in W]
        self._need(q, self._deps(R, W))
        if stream not in self.dsem:
            self.dsem[stream] = self.nc.alloc_semaphore("dsem_" + stream)
            self.dcnt[stream] = 0
        ins = self.e[q].dma_start(out=out, in_=in_, **kw)
        self.dcnt[stream] += 16
        ins.then_inc(self.dsem[stream], 16)
        self._mark((stream, self.dcnt[stream], None), R, W)
        self.ninst += 1

    def finish(self, eng="sp"):
        deps = [(s, v, None, True) for s, v in self.dcnt.items() if v > 0]
        deps += [(k, v, k, True) for k, v in self.cnt.items() if v > 0 and k != eng]
        self._need(eng, deps)


class _Stop(Exception):
    pass


def build(NT, dbg=False, stop=None):
    try:
        return _build(NT, dbg, stop)
    except _Stop as e:
        nc, em = e.args
        em.finish("sp")
        print("STOPPED at", stop, "instructions", em.ninst)
        return nc


def _build(NT, dbg=False, stop=None):
    nc = bass.Bass("TRN2", target_bir_lowering=False)
    S = NT * 512
    x_d = nc.dram_tensor("x", [S, D], F32, kind="ExternalInput")
    wst_d = nc.dram_tensor("wst", [L * NBLK * 256, 2048], F32, kind="ExternalInput")
    prm_d = nc.dram_tensor("prm", [128, NPRM], F32, kind="ExternalInput")
    y_d = nc.dram_tensor("y", [S, D], F32, kind="ExternalOutput")
    wsb_d = nc.dram_tensor("wsb", [L * NBLK, 128, 4096], BF16)
    rope_d = nc.dram_tensor("rope", [NT, 128, 256], F32)
    if dbg:
        dbg_d = nc.dram_tensor("dbg", [NT * L, 128, 8192], BF16, kind="ExternalOutput")
    em = Em(nc)
    if dbg:
        dbg2_d = nc.dram_tensor("dbg2", [16, 128, 512], F32, kind="ExternalOutput")
    ddn = [0]

    def dd(ap, keys, bf=False):
        if not dbg or ddn[0] >= 16:
            return
        i = ddn[0]
        ddn[0] += 1
        if bf:
            em.op("dve", lambda e: e.tensor_copy(ddt[:], ap), keys, ["ddt"])
            em.dma("sp", dbg2_d.ap()[i], ddt[:], "dbg2", R=["ddt"])
        else:
            em.dma("sp", dbg2_d.ap()[i], ap, "dbg2", R=keys)
        print("dd slot", i, keys)

    def SB(name, shape, dt=F32):
        return nc.alloc_sbuf_tensor("s_" + name, shape, dt)

    ddt = SB("ddt", [128, 512]) if dbg else None
    prm = SB("prm", [128, NPRM])
    xres = SB("xres", [128, 4, D])
    xs = SB("xs", [128, D], BF16)
    hnT = SB("hnT", [128, 8, 512], BF16)
    wsl = [SB(f"wsl{i}", [128, 4096], BF16) for i in range(NSLOT)]
    yT = SB("yT", [128, 4, 4, 512], BF16)
    arena = SB("arena", [128, 6144], BF16)
    FR = [SB(f"fr{i}", [128, 512]) for i in range(8)]
    HR = [SB(f"hr{i}", [128, 512], BF16) for i in range(10)]
    big = SB("big", [128, 1024])
    HR2 = [SB(f"hrb{i}", [128, 512], BF16) for i in range(8)]
    FR2 = [SB(f"frb{i}", [128, 512]) for i in range(7)]
    big2 = SB("big2", [128, 1024])
    tiny2 = SB("tiny2", [128, 128])
    arenaB = SB("arenaB", [128, 4096], BF16)
    FRP = SB("frp", [128, 512])
    ident = SB("ident", [128, 128], BF16)
    Vm = SB("Vm", [128, 128])
    Um = SB("Um", [128, 128])
    ones = SB("ones", [128, 128])
    Vb = SB("Vb", [128, 128], BF16)
    onesb = SB("onesb", [128, 2], BF16)
    cst = SB("cst", [128, 64])
    sm = SB("sm", [128, 4, 16])
    sp_ = SB("sp", [128, 4, 12])
    adt = SB("adt", [128, 4, 8])
    tiny = SB("tiny", [128, 128])
    ropeT = SB("ropeT", [128, 4, 64])
    wsmb = SB("wsmb", [128, L, 8, 16], BF16)
    lay = SB("lay", [128, L, 32])
    a_halo = SB("a_halo", [128, L, 4, 3])
    a_h = SB("a_h", [128, L, 4])
    b_halo = SB("b_halo", [128, L, 8, 3])
    b_st = SB("b_st", [128, L, 512])
    b_stb = SB("b_stb", [128, L, 512], BF16)
    c_st = SB("c_st", [128, L, 4, 130])
    c_stb = SB("c_stb", [128, L, 4, 130], BF16)
    d_st = SB("d_st", [128, L, 2, 128])
    d_stb = SB("d_stb", [128, L, 2, 128], BF16)
    print("sbuf remaining", nc.sbuf_bytes_remaining)

    PS = [nc.alloc_psum_tensor(f"ps{i}", [128, 512], F32) for i in range(8)]
    def mk_bank(lo, hi):
        st = [0]

        def f():
            i = lo + st[0] % (hi - lo)
            st[0] += 1
            g = em.gen.get(f"ps{i}", 0) + 1
            em.gen[f"ps{i}"] = g
            return PS[i], f"ps{i}#{g}"
        return f

    bank = mk_bank(0, 8)
    BANKF = [mk_bank(0, 4), mk_bank(4, 8)]

    def P(name, n=None):
        o, m = PRM_OFF[name]
        return prm[:, o:o + (m if n is None else n)]

    def TT(eng, out, a, b, op, R, W):
        em.op(eng, lambda e: e.tensor_tensor(out=out, in0=a, in1=b, op=op), R, W)

    def TS(eng, out, a, s1, s2, op0, op1, R, W):
        if s2 is None:
            em.op(eng, lambda e: e.tensor_scalar(out, a, s1, None, op0), R, W)
        else:
            em.op(eng, lambda e: e.tensor_scalar(out, a, s1, s2, op0, op1), R, W)

    def STT(eng, out, a, s, b, op0, op1, R, W):
        em.op(eng, lambda e: e.scalar_tensor_tensor(out=out, in0=a, scalar=s, in1=b, op0=op0, op1=op1), R, W)

    def CP(eng, out, a, R, W):
        if eng == "act":
            em.op(eng, lambda e: e.copy(out, a), R, W)
        else:
            em.op(eng, lambda e: e.tensor_copy(out, a), R, W)

    def ACT(out, a, func, R, W, bias=None, scale=None, accum=None):
        kw = {}
        if bias is not None:
            kw["bias"] = bias
        if scale is not None:
            kw["scale"] = scale
        if accum is not None:
            kw["accum_out"] = accum
        em.op("act", lambda e: e.activation(out=out, in_=a, func=func, **kw), R, W)

    def MM(out, lhsT, rhs, start, stop, R, W):
        em.op("pe", lambda e: e.matmul(out, lhsT=lhsT, rhs=rhs, start=start, stop=stop), R, W)

    def TR(out, a, R, W):
        em.op("pe", lambda e: e.transpose(out, a, ident[:]), R + ["ident"], W)

    def MS(eng, ap, val, W):
        em.op(eng, lambda e: e.memset(ap, val), [], W)

    class WS:
        def __init__(self):
            self.nissued = 0
            self.done = set()
            self.total = NT * L * NBLK

        def pump(self):
            while self.nissued < self.total:
                j = self.nissued
                if j >= NSLOT and (j - NSLOT) not in self.done:
                    break
                s = j % NSLOT
                lb = j % (L * NBLK)
                em.dma("sp", wsl[s][:], wsb_d.ap()[lb], f"w{s}", R=[f"wsb{lb}"], W=[f"wsl{s}"])
                self.nissued += 1

        def use(self, t, l, b):
            j = (t * L + l) * NBLK + b
            assert j < self.nissued, (j, self.nissued)
            assert j not in self.done
            s = j % NSLOT
            return wsl[s], f"wsl{s}"

        def rel(self, t, l, b):
            self.done.add((t * L + l) * NBLK + b)
            self.pump()

    ws = WS()

    em.dma("sp", prm[:], prm_d.ap(), "prm", W=["prm"])
    wsb_rows = wsb_d.ap().rearrange("b p (r f) -> b (p r) f", f=2048)
    import os
    for lb in range(0 if os.environ.get('NOCAST') else L * NBLK):
        em.dma("pool", wsb_rows[lb], wst_d.ap()[lb * 256:(lb + 1) * 256, :], f"cast{lb}", W=[f"wsb{lb}"])
    MS("pool", ones[:], 1.0, ["ones"])
    em.op("pool", lambda e: e.affine_select(out=Vm[:], in_=ones[:], pattern=[[1, 128]], compare_op=ALU.is_ge,
                                            fill=0.0, base=0, channel_multiplier=-1), ["ones"], ["Vm"])
    em.op("pool", lambda e: e.affine_select(out=Um[:], in_=ones[:], pattern=[[-1, 128]], compare_op=ALU.is_gt,
                                            fill=0.0, base=0, channel_multiplier=1), ["ones"], ["Um"])
    em.op("pool", lambda e: e.affine_select(out=ident[:], in_=ones[:], pattern=[[1, 128]], compare_op=ALU.is_equal,
                                            fill=0.0, base=0, channel_multiplier=-1), ["ones"], ["ident"])
    CP("pool", Vb[:], Vm[:], ["Vm"], ["Vb"])
    MS("pool", onesb[:], 1.0, ["onesb"])
    for (t_, k_) in ((a_halo, "a_halo"), (a_h, "a_h"), (b_halo, "b_halo"), (b_st, "b_st"), (b_stb, "b_stb"),
                     (c_st, "c_st"), (c_stb, "c_stb"), (d_st, "d_st"), (d_stb, "d_stb")):
        MS("pool", t_[:], 0.0, [k_])
    log_g = [math.log1p(-2.0 ** (-5.0 - h)) for h in range(4)]
    ci = SB("ci", [128, 64], I32)
    em.op("pool", lambda e: e.iota(ci[:, 0:1], pattern=[[0, 1]], base=1, channel_multiplier=1), [], ["ci"])
    em.op("pool", lambda e: e.iota(ci[:, 16:48], pattern=[[1, 32]], base=0, channel_multiplier=0), [], ["ci"])
    CP("dve", cst[:, 0:1], ci[:, 0:1], ["ci"], ["cst"])
    CP("dve", cst[:, 16:48], ci[:, 16:48], ["ci"], ["cst"])
    for h in range(4):
        ACT(cst[:, 1 + h:2 + h], cst[:, 0:1], AF.Exp, ["cst"], ["cst"], scale=log_g[h])
        ACT(cst[:, 5 + h:6 + h], cst[:, 0:1], AF.Exp, ["cst"], ["cst"], scale=-log_g[h])
    TS("dve", cst[:, 5:9], cst[:, 5:9], 0.125, None, ALU.mult, None, ["cst"], ["cst"])
    for j in range(2):
        MS("pool", cst[0:64, 9 + j:10 + j], math.exp(128.0 * log_g[2 * j]), ["cst"])
        MS("pool", cst[64:128, 9 + j:10 + j], math.exp(128.0 * log_g[2 * j + 1]), ["cst"])
    ACT(cst[:, 16:48], cst[:, 16:48], AF.Exp, ["cst"], ["cst"], scale=-math.log(10000.0) / 32.0)
    for l in range(L):
        ACT(lay[:, l, 0:4], P(f"a_lam{l}"), AF.Exp, ["prm"], ["lay"], scale=-1.0)
        ACT(lay[:, l, 0:4], lay[:, l, 0:4], AF.Ln, ["lay"], ["lay"], bias=1.0)
        TS("dve", lay[:, l, 4:8], lay[:, l, 0:4], -16.0, None, ALU.mult, None, ["lay"], ["lay"])
        TS("dve", lay[:, l, 0:4], lay[:, l, 0:4], -8.0, None, ALU.mult, None, ["lay"], ["lay"])
        ACT(lay[:, l, 8:16], P(f"b_alog{l}"), AF.Exp, ["prm"], ["lay"])
        TS("dve", lay[:, l, 8:16], lay[:, l, 8:16], -1.0, None, ALU.mult, None, ["lay"], ["lay"])
        CP("dve", wsmb[:, l], P(f"wsm{l}").rearrange("p (k c) -> p k c", c=16), ["prm"], ["wsmb"])
    TWO_PI = 2.0 * math.pi
    C1 = 6.28125
    C2 = TWO_PI - C1
    posi = SB("posi", [128, 4], I32)
    def rope_gen(t, RG, RK):
        em.op("pool", lambda e: e.iota(posi[:], pattern=[[128, 4]], base=t * 512, channel_multiplier=1), ["posi"], ["posi"])
        posf = tiny[:, 0:4]
        CP("dve", posf, posi[:], ["posi"], ["tiny"])
        ang = RG[0][:, 0:128].rearrange("p (b i) -> p b i", i=32)
        TT("dve", ang, posf.unsqueeze(2).to_broadcast([128, 4, 32]),
           cst[:, 16:48].unsqueeze(1).to_broadcast([128, 4, 32]), ALU.mult, ["tiny", "cst"], [RK[0]])
        for which, off in ((1, 0.0), (0, math.pi / 2)):
            a2 = RG[1][:, 0:128].rearrange("p (b i) -> p b i", i=32)
            kf = RG[2][:, 0:128].rearrange("p (b i) -> p b i", i=32)
            ki = RG[3][:, 0:128].bitcast(I32).rearrange("p (b i) -> p b i", i=32)
            TS("dve", a2, ang, off, None, ALU.add, None, [RK[0]], [RK[1]])
            TS("dve", kf, a2, 1.0 / TWO_PI, None, ALU.mult, None, [RK[1]], [RK[2]])
            CP("dve", ki, kf, [RK[2]], [RK[3]])
            CP("dve", kf, ki, [RK[3]], [RK[2]])
            STT("dve", a2, kf, -C1, a2, ALU.mult, ALU.add, [RK[2], RK[1]], [RK[1]])
            STT("dve", a2, kf, -C2, a2, ALU.mult, ALU.add, [RK[2], RK[1]], [RK[1]])
            TS("dve", a2, a2, 3.14159, -3.14159, ALU.min, ALU.max, [RK[1]], [RK[1]])
            ACT(ropeT[:, :, which * 32:(which + 1) * 32], a2, AF.Sin, [RK[1]], ["ropeT"])
        em.dma("sp", rope_d.ap()[t].rearrange("p (b i) -> p b i", i=64), ropeT[:], "ropest", R=["ropeT"], W=[f"rope{t}"])

    rope_gen(0, FR[0:4], ["fr0", "fr1", "fr2", "fr3"])

    cur = [0, 0]

    def chk(name):
        if stop == name:
            if dbg:
                em.dma("sp", dbg_d.ap()[cur[0] * L + cur[1]], yT[:].rearrange("p a b c -> p (a b c)"), "dbg",
                       R=["yT0", "yT1", "yT2", "yT3"])
            raise _Stop(nc, em)

    chk("prologue")
    ws.pump()

    def proj_fm(wt, wk, chunk, pt, pk, ncols=512, c0=0):
        for kc in range(8):
            MM(pt[:, 0:ncols], wt[:, kc * 512 + chunk * 128: kc * 512 + chunk * 128 + 128], hnT[:, kc, c0:c0 + ncols],
               kc == 0, kc == 7, [wk, "hnT"], [pk])

    def proj_tm(wt, wk, blk, pt, pk, ncols=512):
        for kc in range(8):
            MM(pt[:, 0:ncols], hnT[:, kc, blk * 128:(blk + 1) * 128], wt[:, kc * 512: kc * 512 + ncols],
               kc == 0, kc == 7, [wk, "hnT"], [pk])

    def rstd_from_ss(out, ss, n, width, R, W):
        TS("dve", out, ss, 1.0 / n, EPS, ALU.mult, ALU.add, R, W)
        ACT(out, out, AF.Sqrt, W, W)
        em.op("dve", lambda e: e.reciprocal(out=out, in_=out), W, W)

    HRS, FRS, BIGS, TINYS = [HR, HR2], [FR, FR2], [big, big2], [tiny, tiny2]

    BANKF3 = [mk_bank(0, 3), mk_bank(3, 6)]
    PBANK = mk_bank(6, 8)
    CURB = list(BANKF)

    def run_pairs(genf, extra=None):
        CURB[:] = BANKF3 if extra is not None else BANKF
        _run_pairs(genf, extra)
        if extra is not None:
            em.sfx = ""
            for _ in extra:
                pass

    def _run_pairs(genf, extra):
        for a in (0, 2):
            gens = [genf(a, 0), genf(a + 1, 1)]
            sfx = ["", "_b"]
            alive = [True, True]
            blocked1 = False
            upd0 = False
            while any(alive):
                if extra is not None:
                    em.sfx = ""
                    next(extra, None)
                for i in (0, 1):
                    if not alive[i]:
                        continue
                    if i == 1 and blocked1 and not upd0 and alive[0]:
                        continue
                    em.sfx = sfx[i]
                    try:
                        r = next(gens[i])
                    except StopIteration:
                        alive[i] = False
                        continue
                    finally:
                        em.sfx = ""
                    if i == 0 and r == "U":
                        upd0 = True
                    if i == 1 and r == "R":
                        blocked1 = True

    for t in range(NT):
        em.dma("sp", xres[:], x_d.ap()[t * 512:(t + 1) * 512, :].rearrange("(b p) d -> p b d", p=128), "xld", W=["xres"])
        em.dma("sp", ropeT[:], rope_d.ap()[t].rearrange("p (b i) -> p b i", i=64), "ropeld", R=[f"rope{t}"], W=["ropeT"])
        for l in range(L):
            cur[0], cur[1] = t, l
            ss = tiny[:, 0:4]
            for blk in range(4):
                ACT(xs[:], xres[:, blk, :], AF.Square, ["xres"], ["xs", "tiny"], accum=tiny[:, blk:blk + 1])
            rstd_from_ss(tiny[:, 4:8], ss, float(D), 4, ["tiny"], ["tiny"])
            for blk in range(4):
                ACT(xs[:], xres[:, blk, :], AF.Copy, ["xres", "tiny"], ["xs"], scale=tiny[:, 4 + blk:5 + blk])
                pt, pk = bank()
                ptb = pt[:].bitcast(BF16)
                for kc in range(8):
                    TR(ptb[:, kc * 128:(kc + 1) * 128], xs[:, kc * 128:(kc + 1) * 128], ["xs"], [pk])
                TT("dve", hnT[:, :, blk * 128:(blk + 1) * 128], ptb.rearrange("p (k t) -> p k t", t=128),
                   P(f"g{l}").unsqueeze(2).to_broadcast([128, 8, 128]), ALU.mult, [pk, "prm"], ["hnT"])
            chk("phase0")
            pt, pk = bank()
            for blk in range(4):
                for kc in range(8):
                    MM(pt[:, blk * 16:(blk + 1) * 16], hnT[:, kc, blk * 128:(blk + 1) * 128], wsmb[:, l, kc, :],
                       kc == 0, kc == 7, ["hnT", "wsmb"], [pk])
            TT("dve", sm[:], pt[:, 0:64].rearrange("p (b c) -> p b c", c=16),
               P(f"smb{l}").unsqueeze(1).to_broadcast([128, 4, 16]), ALU.add, [pk, "prm"], ["sm"])
            CP("dve", sp_[:, :, 0:8], sm[:, :, 0:8], ["sm"], ["sp"])
            TS("dve", sp_[:, :, 8:12], sm[:, :, 12:16], -1.0, None, ALU.mult, None, ["sm"], ["sp"])
            ACT(sp_[:], sp_[:], AF.Exp, ["sp"], ["sp"])
            ACT(sp_[:], sp_[:], AF.Ln, ["sp"], ["sp"], bias=1.0)
            TT("dve", adt[:], sp_[:, :, 0:8], lay[:, l, 8:16].unsqueeze(1).to_broadcast([128, 4, 8]), ALU.mult,
               ["sp", "lay"], ["adt"])

            chk("small")
            wax, kax = ws.use(t, l, B_AX)
            waz, kaz = ws.use(t, l, B_AZ)
            def genA(c, ch):
                HR, FR, big, tiny = HRS[ch], FRS[ch], BIGS[ch], TINYS[ch]
                bank = CURB[ch]
                xa = big[:, 0:515]
                pt, pk = bank()
                proj_fm(wax, kax, c, pt, pk)
                CP("dve", xa[:, 0:3], a_halo[:, l, c, :], ["a_halo"], ["big"])
                CP("act", xa[:, 3:515], pt[:], [pk], ["big"])
                CP("dve", a_halo[:, l, c, :], xa[:, 512:515], ["big"], ["a_halo"])
                yield ""
                cw = P(f"a_cw{l}")
                xc = FR[0]
                TS("dve", xc[:], xa[:, 3:515], cw[:, c * 4 + 3:c * 4 + 4], P(f"a_cb{l}")[:, c:c + 1], ALU.mult, ALU.add,
                   ["big", "prm"], ["fr0"])
                for j in range(3):
                    STT("dve", xc[:], xa[:, j:j + 512], cw[:, c * 4 + j:c * 4 + j + 1], xc[:], ALU.mult, ALU.add,
                        ["big", "prm", "fr0"], ["fr0"])
                pr, prk = bank()
                MM(pr[:], P(f"a_wa{l}")[:, c * 128:(c + 1) * 128], xc[:], True, True, ["prm", "fr0"], [prk])
                pi, pik = bank()
                MM(pi[:], P(f"a_wx{l}")[:, c * 128:(c + 1) * 128], xc[:], True, True, ["prm", "fr0"], [pik])
                yield ""
                r_, i_, a_, s_ = FR[1], FR[2], FR[3], FR[4]
                ACT(r_[:], pr[:], AF.Sigmoid, [prk, "prm"], ["fr1"], bias=P(f"a_ba{l}")[:, c:c + 1])
                ACT(i_[:], pi[:], AF.Sigmoid, [pik, "prm"], ["fr2"], bias=P(f"a_bx{l}")[:, c:c + 1])
                yield ""
                ACT(a_[:], r_[:], AF.Exp, ["fr1", "lay"], ["fr3"], scale=lay[:, l, c:c + 1])
                ACT(s_[:], r_[:], AF.Exp, ["fr1", "lay"], ["fr4"], scale=lay[:, l, 4 + c:5 + c])
                ACT(s_[:], s_[:], AF.Sqrt, ["fr4"], ["fr4"], bias=1.0, scale=-1.0)
                yield ""
                TT("dve", i_[:], i_[:], xc[:], ALU.mult, ["fr2", "fr0"], ["fr2"])
                TT("dve", i_[:], i_[:], s_[:], ALU.mult, ["fr2", "fr4"], ["fr2"])
                yield ""
                h_ = FR[5]
                em.op("dve", lambda e: e.tensor_tensor_scan(out=h_[:], data0=a_[:], data1=i_[:], initial=a_h[:, l, c:c + 1],
                                                            op0=ALU.mult, op1=ALU.add), ["fr3", "fr2", "a_h"], ["fr5"])
                CP("dve", a_h[:, l, c:c + 1], h_[:, 511:512], ["fr5"], ["a_h"])
                yield ""
                pz, pzk = bank()
                proj_fm(waz, kaz, c, pz, pzk)
                zs = FR[6]
                ACT(zs[:], pz[:], AF.Silu, [pzk], ["fr6"])
                yield ""
                TT("dve", yT[:, 0, c, :], h_[:], zs[:], ALU.mult, ["fr5", "fr6"], ["yT0"])
                if c == 3 and t == 0 and l == 0:
                    dd(hnT[:, 0, :], ["hnT"], True)
                    dd(xc[:], ["fr0"])
                    dd(r_[:], ["fr1"])
                    dd(a_[:], ["fr3"])
                    dd(i_[:], ["fr2"])
                    dd(h_[:], ["fr5"])
                    dd(zs[:], ["fr6"])
                    dd(yT[:, 0, c, :], ["yT0"], True)
                yield ""

            run_pairs(genA)

            ws.rel(t, l, B_AX)
            ws.rel(t, l, B_AZ)

            chk("A")
            xbcT = arenaB[:, 0:4096].rearrange("p (c t) -> p c t", t=512)
            for c8 in range(8):
                wb_, kb_ = ws.use(t, l, B_XBC0 + c8 // 4)
                xa = big[:, 0:515]
                pt, pk = bank()
                proj_fm(wb_, kb_, c8 % 4, pt, pk)
                if c8 % 4 == 3:
                    ws.rel(t, l, B_XBC0 + c8 // 4)
                CP("dve", xa[:, 0:3], b_halo[:, l, c8, :], ["b_halo"], ["big"])
                CP("act", xa[:, 3:515], pt[:], [pk], ["big"])
                CP("dve", b_halo[:, l, c8, :], xa[:, 512:515], ["big"], ["b_halo"])
                cw = P(f"b_cw{l}")
                xc = FR[c8 % 2]
                xk = f"fr{c8 % 2}"
                TS("dve", xc[:], xa[:, 3:515], cw[:, c8 * 4 + 3:c8 * 4 + 4], P(f"b_cb{l}")[:, c8:c8 + 1], ALU.mult, ALU.add,
                   ["big", "prm"], [xk])
                for j in range(3):
                    STT("dve", xc[:], xa[:, j:j + 512], cw[:, c8 * 4 + j:c8 * 4 + j + 1], xc[:], ALU.mult, ALU.add,
                        ["big", "prm", xk], [xk])
                ACT(xbcT[:, c8, :], xc[:], AF.Silu, [xk], ["arenaB"])
            wz, kz = ws.use(t, l, B_BZ)
            def genB(blk, ch):
                HR, FR, big, tiny = HRS[ch], FRS[ch], BIGS[ch], TINYS[ch]
                bank = CURB[ch]
                bs = slice(blk * 128, (blk + 1) * 128)
                pt, pk = bank()
                ptb = pt[:].bitcast(BF16)
                for c6 in range(6):
                    TR(ptb[:, c6 * 128:(c6 + 1) * 128], xbcT[:, c6, bs], ["arenaB"], [pk])
                xdt, xD, Btm = HR[0], HR[1], HR[2]
                TT("dve", xdt[:].rearrange("p (e q) -> p e q", q=64), ptb[:, 0:512].rearrange("p (e q) -> p e q", q=64),
                   sp_[:, blk, 0:8].unsqueeze(2).to_broadcast([128, 8, 64]), ALU.mult, [pk, "sp"], ["hr0"])
                TT("dve", xD[:].rearrange("p (e q) -> p e q", q=64), ptb[:, 0:512].rearrange("p (e q) -> p e q", q=64),
                   P(f"b_dsk{l}").unsqueeze(2).to_broadcast([128, 8, 64]), ALU.mult, [pk, "prm"], ["hr1"])
                CP("act", Btm[:, 0:256], ptb[:, 512:768], [pk], ["hr2"])
                yield ""
                pm, pmk = bank()
                MM(pm[:, 0:8], Vm[:], adt[:, blk, :], True, True, ["Vm", "adt"], [pmk])
                MM(pm[:, 8:16], ones[:], adt[:, blk, :], True, True, ["ones", "adt"], [pmk])
                lhsE = big[:].rearrange("p (e s) -> p e s", s=128)
                TT("dve", lhsE, Um[:].unsqueeze(1).to_broadcast([128, 8, 128]),
                   adt[:, blk, :].unsqueeze(2).to_broadcast([128, 8, 128]), ALU.mult, ["Um", "adt"], ["big"])
                yield ""
                acs = tiny[:, 16:24]
                atot = tiny[:, 24:32]
                CP("dve", tiny[:, 16:32], pm[:, 0:16], [pmk], ["tiny"])
                ea, dte, cd = tiny[:, 32:40], tiny[:, 40:48], tiny[:, 48:56]
                ACT(ea, acs, AF.Exp, ["tiny"], ["tiny"])
                TT("dve", dte, atot, acs, ALU.subtract, ["tiny"], ["tiny"])
                ACT(dte, dte, AF.Exp, ["tiny"], ["tiny"])
                ACT(cd, atot, AF.Exp, ["tiny"], ["tiny"])
                yield ""
                pd0, pd0k = bank()
                pd1, pd1k = bank()
                for e8 in range(8):
                    pd, pdk = (pd0, pd0k) if e8 < 4 else (pd1, pd1k)
                    MM(pd[:, (e8 % 4) * 128:(e8 % 4 + 1) * 128], lhsE[:, e8, :], Vm[:], True, True, ["big", "Vm"], [pdk])
                yield ""
                Lm = HR[3], HR[4]
                ACT(Lm[0][:], pd0[:], AF.Exp, [pd0k], ["hr3"])
                ACT(Lm[1][:], pd1[:], AF.Exp, [pd1k], ["hr4"])
                pcb, pcbk = bank()
                for g in range(2):
                    MM(pcb[:, g * 128:(g + 1) * 128], xbcT[:, 4 + g, bs], xbcT[:, 6 + g, bs], True, True, ["arenaB"], [pcbk])
                yield ""
                cbm = HR[5]
                TT("dve", cbm[:, 0:256].rearrange("p (g s) -> p g s", s=128), pcb[:, 0:256].rearrange("p (g s) -> p g s", s=128),
                   Vb[:].unsqueeze(1).to_broadcast([128, 2, 128]), ALU.mult, [pcbk, "Vb"], ["hr5"])
                xdd = HR[7]
                TT("dve", xdd[:].rearrange("p (e q) -> p e q", q=64), xdt[:].rearrange("p (e q) -> p e q", q=64),
                   dte.unsqueeze(2).to_broadcast([128, 8, 64]), ALU.mult, ["hr0", "tiny"], ["hr7"])
                yield ""
                for g in range(2):
                    TT("dve", Lm[g][:].rearrange("p (e s) -> p e s", s=128), Lm[g][:].rearrange("p (e s) -> p e s", s=128),
                       cbm[:, g * 128:(g + 1) * 128].unsqueeze(1).to_broadcast([128, 4, 128]), ALU.mult,
                       [f"hr{3 + g}", "hr5"], [f"hr{3 + g}"])
                yield ""
                py, pyk = bank()
                for e8 in range(8):
                    MM(py[:, e8 * 64:(e8 + 1) * 64], Lm[e8 // 4][:, (e8 % 4) * 128:(e8 % 4 + 1) * 128], xdt[:, e8 * 64:(e8 + 1) * 64],
                       True, True, [f"hr{3 + e8 // 4}", "hr0"], [pyk])
                pz, pzk = bank()
                proj_tm(wz, kz, blk, pz, pzk)
                zs = FR[3]
                ACT(zs[:], pz[:], AF.Silu, [pzk], ["fr3"])
                yield "R"
                pyo, pyok = bank()
                for g in range(2):
                    MM(pyo[:, g * 256:(g + 1) * 256], xbcT[:, 6 + g, bs], b_stb[:, l, g * 256:(g + 1) * 256], True, True,
                       ["arenaB", "b_stb"], [pyok])
                t1 = FR[2]
                TT("dve", t1[:].rearrange("p (e q) -> p e q", q=64), pyo[:].rearrange("p (e q) -> p e q", q=64),
                   ea.unsqueeze(2).to_broadcast([128, 8, 64]), ALU.mult, [pyok, "tiny"], ["fr2"])
                TT("dve", t1[:], t1[:], py[:], ALU.add, ["fr2", pyk], ["fr2"])
                pst, pstk = bank()
                for g in range(2):
                    MM(pst[:, g * 256:(g + 1) * 256], Btm[:, g * 128:(g + 1) * 128], xdd[:, g * 256:(g + 1) * 256], True, True,
                       ["hr2", "hr7"], [pstk])
                TT("dve", b_st[:, l, :].rearrange("p (e q) -> p e q", q=64), b_st[:, l, :].rearrange("p (e q) -> p e q", q=64),
                   cd.unsqueeze(2).to_broadcast([128, 8, 64]), ALU.mult, ["b_st", "tiny"], ["b_st"])
                TT("dve", b_st[:, l, :], b_st[:, l, :], pst[:], ALU.add, ["b_st", pstk], ["b_st"])
                CP("act", b_stb[:, l, :], b_st[:, l, :], ["b_st"], ["b_stb"])
                yield "U"
                TT("dve", t1[:], t1[:], xD[:], ALU.add, ["fr2", "hr1"], ["fr2"])
                yield ""
                TT("dve", t1[:], t1[:], zs[:], ALU.mult, ["fr2", "fr3"], ["fr2"])
                ACT(zs[:], t1[:], AF.Square, ["fr2"], ["fr3", "tiny"], accum=tiny[:, 56:57])
                yield ""
                rstd_from_ss(tiny[:, 57:58], tiny[:, 56:57], 512.0, 1, ["tiny"], ["tiny"])
                yn = HR[6]
                ACT(yn[:], t1[:], AF.Copy, ["fr2", "tiny"], ["hr6"], scale=tiny[:, 57:58])
                yield ""
                pt2, pt2k = bank()
                pt2b = pt2[:].bitcast(BF16)
                for c in range(4):
                    TR(pt2b[:, c * 128:(c + 1) * 128], yn[:, c * 128:(c + 1) * 128], ["hr6"], [pt2k])
                TT("dve", yT[:, 1, :, bs], pt2b[:, 0:512].rearrange("p (c t) -> p c t", t=128),
                   P(f"b_g{l}").unsqueeze(2).to_broadcast([128, 4, 128]), ALU.mult, [pt2k, "prm"], ["yT1"])
                yield ""

            qT = arena[:, 0:2048].rearrange("p (h t) -> p h t", t=512)
            kT = arena[:, 2048:4096].rearrange("p (h t) -> p h t", t=512)
            zsT = arena[:, 4096:6144].rearrange("p (h t) -> p h t", t=512)
            WB = {}

            def genCprep():
                bank = PBANK
                wq, kq = ws.use(t, l, B_CQ)
                wk_, kk = ws.use(t, l, B_CK)
                WB["ck"] = (wk_, kk)
                for h in range(4):
                    pt, pk = bank()
                    proj_fm(wq, kq, h, pt, pk)
                    CP("act", qT[:, h, :], pt[:], [pk], ["arena"])
                    yield ""
                    pt, pk = bank()
                    proj_fm(wk_, kk, h, pt, pk)
                    em.op("act", lambda e: e.mul(kT[:, h, :], pt[:], 128.0 ** -0.5), [pk], ["arena"])
                    yield ""
                ws.rel(t, l, B_CQ)
                wzc, kzc = ws.use(t, l, B_CZ)
                for c in range(4):
                    pt, pk = bank()
                    proj_fm(wzc, kzc, c, pt, pk)
                    ACT(FRP[:], pt[:], AF.Silu, [pk], ["frp"])
                    TS("pool", zsT[:, c, :], FRP[:], P(f"c_g{l}")[:, c:c + 1], None, ALU.mult, None, ["frp", "prm"], ["arena"])
                    yield ""
                ws.rel(t, l, B_CZ)

            run_pairs(genB, extra=genCprep())
            ws.rel(t, l, B_BZ)

            chk("B")
            wk_, kk = WB["ck"]
            wv, kv = ws.use(t, l, B_CV)
            wo, ko = ws.use(t, l, B_CO)
            def genC(blk, ch):
                HR, FR, big, tiny = HRS[ch], FRS[ch], BIGS[ch], TINYS[ch]
                bank = CURB[ch]
                bs = slice(blk * 128, (blk + 1) * 128)
                kTM, vTM, so = HR[0], HR[1], HR[2]
                pt, pk = bank()
                proj_tm(wk_, kk, blk, pt, pk)
                em.op("act", lambda e: e.mul(kTM[:], pt[:], 128.0 ** -0.5), [pk], ["hr0"])
                yield ""
                pt, pk = bank()
                proj_tm(wv, kv, blk, pt, pk)
                CP("act", vTM[:], pt[:], [pk], ["hr1"])
                yield ""
                pt, pk = bank()
                proj_tm(wo, ko, blk, pt, pk)
                ACT(so[:], pt[:], AF.Sigmoid, [pk], ["hr2"])
                yield ""
                lnf = sp_[:, blk, 8:12]
                logi = sm[:, blk, 8:12]
                pm, pmk = bank()
                MM(pm[:, 0:4], Vm[:], lnf, True, True, ["Vm", "sp"], [pmk])
                MM(pm[:, 4:8], ones[:], lnf, True, True, ["ones", "sp"], [pmk])
                lhsC = big[:, 0:512].rearrange("p (h s) -> p h s", s=128)
                STT("dve", lhsC, lnf.unsqueeze(2).to_broadcast([128, 4, 128]), -1.0,
                    Um[:].unsqueeze(1).to_broadcast([128, 4, 128]), ALU.mult, ALU.mult, ["Um", "sp"], ["big"])
                yield ""
                Fn, Ftn = tiny[:, 16:20], tiny[:, 20:24]
                CP("dve", tiny[:, 16:24], pm[:, 0:8], [pmk], ["tiny"])
                eF, eFt, wend = tiny[:, 24:28], tiny[:, 28:32], tiny[:, 32:36]
                ACT(eF, Fn, AF.Exp, ["tiny"], ["tiny"], scale=-1.0)
                ACT(eFt, Ftn, AF.Exp, ["tiny"], ["tiny"], scale=-1.0)
                TT("dve", wend, Fn, Ftn, ALU.subtract, ["tiny"], ["tiny"])
                TT("dve", wend, wend, logi, ALU.add, ["tiny", "sm"], ["tiny"])
                ACT(wend, wend, AF.Exp, ["tiny"], ["tiny"])
                yield ""
                pdf, pdfk = bank()
                for h in range(4):
                    MM(pdf[:, h * 128:(h + 1) * 128], lhsC[:, h, :], Vm[:], True, True, ["big", "Vm"], [pdfk])
                psc, psck = bank()
                for h in range(4):
                    MM(psc[:, h * 128:(h + 1) * 128], kT[:, h, bs], qT[:, h, bs], True, True, ["arena"], [psck])
                yield ""
                Dt = HR[3]
                for h in range(4):
                    ACT(Dt[:, h * 128:(h + 1) * 128], pdf[:, h * 128:(h + 1) * 128], AF.Exp, [pdfk, "sm"], ["hr3"],
                        bias=logi[:, h:h + 1])
                kw = HR[6]
                TT("dve", kw[:].rearrange("p (h s) -> p h s", s=128), kTM[:].rearrange("p (h s) -> p h s", s=128),
                   wend.unsqueeze(2).to_broadcast([128, 4, 128]), ALU.mult, ["hr0", "tiny"], ["hr6"])
                yield ""
                TT("dve", Dt[:].rearrange("p (h s) -> p h s", s=128), Dt[:].rearrange("p (h s) -> p h s", s=128),
                   Vb[:].unsqueeze(1).to_broadcast([128, 4, 128]), ALU.mult, ["hr3", "Vb"], ["hr3"])
                yield ""
                Pt = HR[4]
                TT("dve", Pt[:], psc[:], Dt[:], ALU.mult, [psck, "hr3"], ["hr4"])
                yield ""
                pnum, pnumk = bank()
                for h in range(4):
                    MM(pnum[:, h * 128:(h + 1) * 128], Pt[:, h * 128:(h + 1) * 128], vTM[:, h * 128:(h + 1) * 128], True, True,
                       ["hr4", "hr1"], [pnumk])
                yield "R"
                pin, pink = bank()
                for h in range(4):
                    MM(pin[:, h * 128:(h + 1) * 128], qT[:, h, bs], c_stb[:, l, h, 0:128], True, True, ["arena", "c_stb"], [pink])
                pden, pdenk = bank()
                for h in range(4):
                    MM(pden[:, 2 * h:2 * h + 2], Pt[:, h * 128:(h + 1) * 128], onesb[:], True, True, ["hr4", "onesb"], [pdenk])
                for h in range(4):
                    MM(pden[:, 8 + 2 * h:10 + 2 * h], qT[:, h, bs], c_stb[:, l, h, 128:130], True, True,
                       ["arena", "c_stb"], [pdenk])
                t1 = FR[1]
                TT("dve", t1[:].rearrange("p (h s) -> p h s", s=128), pin[:].rearrange("p (h s) -> p h s", s=128),
                   eF.unsqueeze(2).to_broadcast([128, 4, 128]), ALU.mult, [pink, "tiny"], ["fr1"])
                TT("dve", t1[:], t1[:], pnum[:], ALU.add, ["fr1", pnumk], ["fr1"])
                den = tiny[:, 36:52]
                CP("dve", den, pden[:, 0:16], [pdenk], ["tiny"])
                pS, pSk = bank()
                for h in range(4):
                    MM(pS[:, h * 128:(h + 1) * 128], kw[:, h * 128:(h + 1) * 128], vTM[:, h * 128:(h + 1) * 128], True, True,
                       ["hr6", "hr1"], [pSk])
                pn, pnk = bank()
                for h in range(4):
                    MM(pn[:, 2 * h:2 * h + 2], kw[:, h * 128:(h + 1) * 128], onesb[:], True, True, ["hr6", "onesb"], [pnk])
                TT("dve", c_st[:, l], c_st[:, l], eFt.unsqueeze(2).to_broadcast([128, 4, 130]), ALU.mult,
                   ["c_st", "tiny"], ["c_st"])
                TT("dve", c_st[:, l, :, 0:128], c_st[:, l, :, 0:128], pS[:].rearrange("p (h s) -> p h s", s=128), ALU.add,
                   ["c_st", pSk], ["c_st"])
                TT("dve", c_st[:, l, :, 128:130], c_st[:, l, :, 128:130], pn[:, 0:8].rearrange("p (h two) -> p h two", two=2),
                   ALU.add, ["c_st", pnk], ["c_st"])
                CP("act", c_stb[:, l], c_st[:, l], ["c_st"], ["c_stb"])
                yield "U"
                dv = den.rearrange("p (a h two) -> p a h two", a=2, two=2)
                dn, rec, r2 = tiny[:, 52:56], tiny[:, 56:60], tiny[:, 60:64]
                TT("dve", dn, dv[:, 1, :, 0], eF, ALU.mult, ["tiny"], ["tiny"])
                TT("dve", dn, dn, dv[:, 0, :, 0], ALU.add, ["tiny"], ["tiny"])
                TS("dve", rec, dn, -1.0, None, ALU.mult, None, ["tiny"], ["tiny"])
                TT("dve", rec, rec, dn, ALU.max, ["tiny"], ["tiny"])
                TS("dve", rec, rec, 1.0, None, ALU.max, None, ["tiny"], ["tiny"])
                em.op("dve", lambda e: e.reciprocal(out=rec, in_=rec), ["tiny"], ["tiny"])
                yield ""
                TT("dve", t1[:], t1[:], so[:], ALU.mult, ["fr1", "hr2"], ["fr1"])
                yield ""
                ssq = tiny[:, 64:68]
                for h in range(4):
                    ACT(FR[2][:, 0:128], t1[:, h * 128:(h + 1) * 128], AF.Square, ["fr1"], ["fr2", "tiny"], accum=ssq[:, h:h + 1])
                yield ""
                TT("dve", r2, rec, rec, ALU.mult, ["tiny"], ["tiny"])
                TT("dve", r2, r2, ssq, ALU.mult, ["tiny"], ["tiny"])
                rstd_from_ss(r2, r2, 128.0, 4, ["tiny"], ["tiny"])
                TT("dve", r2, r2, rec, ALU.mult, ["tiny"], ["tiny"])
                yield ""
                hn = HR[5]
                TT("dve", hn[:].rearrange("p (h s) -> p h s", s=128), t1[:].rearrange("p (h s) -> p h s", s=128),
                   r2.unsqueeze(2).to_broadcast([128, 4, 128]), ALU.mult, ["fr1", "tiny"], ["hr5"])
                pt2, pt2k = bank()
                pt2b = pt2[:].bitcast(BF16)
                for h in range(4):
                    TR(pt2b[:, h * 128:(h + 1) * 128], hn[:, h * 128:(h + 1) * 128], ["hr5"], [pt2k])
                TT("dve", yT[:, 2, :, bs], pt2b[:, 0:512].rearrange("p (c t) -> p c t", t=128), zsT[:, :, bs], ALU.mult,
                   [pt2k, "arena"], ["yT2"])
                yield ""

            zsD = arenaB[:, 0:2048].rearrange("p (h t) -> p h t", t=512)

            def genDprep():
                bank = PBANK
                wdz, kdz = ws.use(t, l, B_DZ)
                for c in range(4):
                    pt, pk = bank()
                    proj_fm(wdz, kdz, c, pt, pk)
                    ACT(FRP[:], pt[:], AF.Silu, [pk], ["frp"])
                    TS("pool", zsD[:, c, :], FRP[:], P(f"d_g{l}")[:, c:c + 1], None, ALU.mult, None, ["frp", "prm"], ["arenaB"])
                    yield ""
                ws.rel(t, l, B_DZ)

            run_pairs(genC, extra=genDprep())
            ws.rel(t, l, B_CK)
            ws.rel(t, l, B_CV)
            ws.rel(t, l, B_CO)

            chk("C")
            wqk, kqk = ws.use(t, l, B_DQK)
            wdv, kdv = ws.use(t, l, B_DV)
            def genD(blk, ch):
                HR, FR, big, tiny = HRS[ch], FRS[ch], BIGS[ch], TINYS[ch]
                bank = CURB[ch]
                bs = slice(blk * 128, (blk + 1) * 128)
                vD = HR[0]
                pt, pk = bank()
                proj_tm(wdv, kdv, blk, pt, pk)
                CP("act", vD[:], pt[:], [pk], ["hr0"])
                yield ""
                pq, pqk = bank()
                proj_tm(wqk, kqk, blk, pq, pqk)
                yield ""
                qv = pq[:].rearrange("p (h two i) -> p h two i", two=2, i=32)
                cosb = ropeT[:, blk, 0:32].unsqueeze(1).to_broadcast([128, 8, 32])
                sinb = ropeT[:, blk, 32:64].unsqueeze(1).to_broadcast([128, 8, 32])
                qr = FR[1][:].rearrange("p (h two i) -> p h two i", two=2, i=32)
                ta = FR[2][:, 0:256].rearrange("p (h i) -> p h i", i=32)
                tb = FR[2][:, 256:512].rearrange("p (h i) -> p h i", i=32)
                TT("dve", qr[:, :, 0, :], qv[:, :, 0, :], cosb, ALU.mult, [pqk, "ropeT"], ["fr1"])
                TT("dve", ta, qv[:, :, 1, :], sinb, ALU.mult, [pqk, "ropeT"], ["fr2"])
                yield ""
                TT("dve", qr[:, :, 0, :], qr[:, :, 0, :], ta, ALU.subtract, ["fr1", "fr2"], ["fr1"])
                TT("dve", qr[:, :, 1, :], qv[:, :, 0, :], sinb, ALU.mult, [pqk, "ropeT"], ["fr1"])
                TT("dve", tb, qv[:, :, 1, :], cosb, ALU.mult, [pqk, "ropeT"], ["fr2"])
                yield ""
                TT("dve", qr[:, :, 1, :], qr[:, :, 1, :], tb, ALU.add, ["fr1", "fr2"], ["fr1"])
                yield ""
                qrs = HR[1]
                TT("dve", qrs[:].rearrange("p (h d) -> p h d", d=64), FR[1][:].rearrange("p (h d) -> p h d", d=64),
                   cst[:, 1:9].unsqueeze(2).to_broadcast([128, 8, 64]), ALU.mult, ["fr1", "cst"], ["hr1"])
                yield ""
                pt2, pt2k = bank()
                pt2b = pt2[:].bitcast(BF16)
                for c in range(4):
                    TR(pt2b[:, c * 128:(c + 1) * 128], qrs[:, c * 128:(c + 1) * 128], ["hr1"], [pt2k])
                qkT = HR[2]
                CP("act", qkT[:], pt2b[:, 0:512], [pt2k], ["hr2"])
                yield ""
                pscs = [bank(), bank()]
                for h in range(4):
                    a_, j_ = h % 2, h // 2
                    pr_ = a_ * 64
                    MM(pscs[a_][0][:, j_ * 128:(j_ + 1) * 128], qkT[pr_:pr_ + 64, (2 + j_) * 128:(3 + j_) * 128],
                       qkT[pr_:pr_ + 64, j_ * 128:(j_ + 1) * 128], True, True, ["hr2"], [pscs[a_][1]])
                yield ""
                Pt = HR[3]
                for a_ in range(2):
                    TT("dve", Pt[:, a_ * 256:(a_ + 1) * 256].rearrange("p (j s) -> p j s", s=128),
                       pscs[a_][0][:, 0:256].rearrange("p (j s) -> p j s", s=128),
                       Vb[:].unsqueeze(1).to_broadcast([128, 2, 128]), ALU.mult, [pscs[a_][1], "Vb"], ["hr3"])
                yield "R"
                pys = [bank(), bank()]
                for h in range(4):
                    a_, j_ = h % 2, h // 2
                    pr_ = a_ * 64
                    MM(pys[a_][0][:, j_ * 128:(j_ + 1) * 128], Pt[:, (a_ * 2 + j_) * 128:(a_ * 2 + j_ + 1) * 128],
                       vD[:, h * 128:(h + 1) * 128], True, False, ["hr3", "hr0"], [pys[a_][1]])
                    MM(pys[a_][0][:, j_ * 128:(j_ + 1) * 128], qkT[pr_:pr_ + 64, j_ * 128:(j_ + 1) * 128],
                       d_stb[pr_:pr_ + 64, l, j_, :], False, True, ["hr2", "d_stb"], [pys[a_][1]])
                pR, pRk = bank()
                for h in range(4):
                    pr_ = (h % 2) * 64
                    MM(pR[pr_:pr_ + 64, (h // 2) * 128:(h // 2 + 1) * 128], qrs[:, (4 + h) * 64:(5 + h) * 64],
                       vD[:, h * 128:(h + 1) * 128], True, True, ["hr1", "hr0"], [pRk])
                TT("dve", d_st[:, l], d_st[:, l], pR[:, 0:256].rearrange("p (j e) -> p j e", e=128), ALU.add,
                   ["d_st", pRk], ["d_st"])
                for j in range(2):
                    TS("dve", d_st[:, l, j, :], d_st[:, l, j, :], cst[:, 9 + j:10 + j], None, ALU.mult, None,
                       ["d_st", "cst"], ["d_st"])
                CP("act", d_stb[:, l], d_st[:, l], ["d_st"], ["d_stb"])
                yield "U"
                ssq = tiny[:, 16:20]
                for h in range(4):
                    a_, j_ = h % 2, h // 2
                    ACT(FR[3][:, 0:128], pys[a_][0][:, j_ * 128:(j_ + 1) * 128], AF.Square, [pys[a_][1]], ["fr3", "tiny"],
                        accum=ssq[:, h:h + 1])
                yield ""
                rstd_from_ss(tiny[:, 20:24], ssq, 128.0, 4, ["tiny"], ["tiny"])
                yield ""
                hn = HR[4]
                for h in range(4):
                    a_, j_ = h % 2, h // 2
                    TS("dve", hn[:, h * 128:(h + 1) * 128], pys[a_][0][:, j_ * 128:(j_ + 1) * 128], tiny[:, 20 + h:21 + h], None,
                       ALU.mult, None, [pys[a_][1], "tiny"], ["hr4"])
                yield ""
                pt3, pt3k = bank()
                pt3b = pt3[:].bitcast(BF16)
                for h in range(4):
                    TR(pt3b[:, h * 128:(h + 1) * 128], hn[:, h * 128:(h + 1) * 128], ["hr4"], [pt3k])
                TT("dve", yT[:, 3, :, bs], pt3b[:, 0:512].rearrange("p (c t) -> p c t", t=128), zsD[:, :, bs], ALU.mult,
                   [pt3k, "arenaB"], ["yT3"])
                yield ""

            run_pairs(genD)
            ws.rel(t, l, B_DQK)
            ws.rel(t, l, B_DV)

            chk("D")
            if dbg:
                em.dma("sp", dbg_d.ap()[t * L + l], yT[:].rearrange("p a b c -> p (a b c)"), "dbg",
                       R=["yT0", "yT1", "yT2", "yT3"])

            if l == L - 1 and t + 1 < NT:
                rope_gen(t + 1, FR[3:7], ["fr3", "fr4", "fr5", "fr6"])
            mT = arena[:, 0:4096].rearrange("p (k t) -> p k t", t=512)
            for jb in range(2):
                for n in range(4):
                    wg, kg = ws.use(t, l, blk_gate(n, jb))
                    wbb, kbb = ws.use(t, l, blk_wb(n // 2, jb))
                    for j in range(4):
                        pg, pgk = bank()
                        proj_fm(wg, kg, j, pg, pgk)
                        pu, puk = bank()
                        for kc in range(4):
                            o_ = (n % 2) * 2048 + kc * 512 + j * 128
                            MM(pu[:], wbb[:, o_:o_ + 128], yT[:, n, kc, :], kc == 0, kc == 3, [kbb, f"yT{n}"], [puk])
                        sg = FR[(n * 4 + j) % 2]
                        sgk = f"fr{(n * 4 + j) % 2}"
                        ACT(sg[:], pg[:], AF.Sigmoid, [pgk], [sgk])
                        if n == 0:
                            TT("dve", FR2[j][:], sg[:], pu[:], ALU.mult, [sgk, puk], [f"fr{j}_b"])
                        else:
                            TT("dve", sg[:], sg[:], pu[:], ALU.mult, [sgk, puk], [sgk])
                            if n < 3:
                                TT("pool", FR2[j][:], FR2[j][:], sg[:], ALU.add, [f"fr{j}_b", sgk], [f"fr{j}_b"])
                            else:
                                TT("pool", mT[:, jb * 4 + j, :], FR2[j][:], sg[:], ALU.add, [f"fr{j}_b", sgk], ["arena"])
                    ws.rel(t, l, blk_gate(n, jb))
                    if n % 2 == 1:
                        ws.rel(t, l, blk_wb(n // 2, jb))
            chk("merge")
            wo0, ko0 = ws.use(t, l, B_WOUT)
            wo1, ko1 = ws.use(t, l, B_WOUT + 1)
            for blk in range(4):
                for hf in range(2):
                    wt_, wk2 = (wo0, ko0) if hf == 0 else (wo1, ko1)
                    po, pok = bank()
                    for kc in range(8):
                        MM(po[:], mT[:, kc, blk * 128:(blk + 1) * 128], wt_[:, kc * 512:(kc + 1) * 512], kc == 0, kc == 7,
                           ["arena", wk2], [pok])
                    TT("dve", xres[:, blk, hf * 512:(hf + 1) * 512], xres[:, blk, hf * 512:(hf + 1) * 512], po[:], ALU.add,
                       ["xres", pok], ["xres"])
            ws.rel(t, l, B_WOUT)
            ws.rel(t, l, B_WOUT + 1)
        for blk in range(4):
            ACT(xs[:], xres[:, blk, :], AF.Square, ["xres"], ["xs", "tiny"], accum=tiny[:, blk:blk + 1])
        rstd_from_ss(tiny[:, 4:8], tiny[:, 0:4], float(D), 4, ["tiny"], ["tiny"])
        for blk in range(4):
            STT("dve", xres[:, blk, :], xres[:, blk, :], tiny[:, 4 + blk:5 + blk], P("gf"), ALU.mult, ALU.mult,
                ["xres", "tiny", "prm"], ["xres"])
        em.dma("sp", y_d.ap()[t * 512:(t + 1) * 512, :].rearrange("(b p) d -> p b d", p=128), xres[:], "yst", R=["xres"])
    em.finish("sp")
    print("instructions", em.ninst, "waits", em.nwait)
    return nc


_CACHE = {}


def run(inp, NT, dbg=False, ncores=8, stop=None):
    x = np.asarray(inp["x"], np.float32)
    B = x.shape[0]
    prm = pack_params(inp)
    wst = pack_weights(inp)
    key = (NT, dbg, stop)
    if key not in _CACHE:
        _CACHE[key] = build(NT, dbg, stop)
    nc = _CACHE[key]
    in_maps = []
    for c in range(ncores):
        b = c % B
        in_maps.append({"x": np.ascontiguousarray(x[b, :NT * 512]), "wst": wst, "prm": prm})
    res = run_bass_kernel_spmd(nc, in_maps, core_ids=list(range(ncores)))
    return res


def kernel(**inputs):
    inp = {k: np.asarray(v) for k, v in inputs.items()}
    NT = inp["x"].shape[1] // 512
    res = run(inp, NT)
    B = inp["x"].shape[0]
    return np.stack([res.results[b]["y"] for b in range(B)], axis=0).astype(np.float32)
```
